# Optimizing a Trainium2 kernel written in Bass

```python
import jax, jax.numpy as jnp
from jax import lax
import numpy as np

D_MODEL = 2048
BATCH = 4
SEQ = 4096
DEPTH = 1

PLE_DIM = 256
M_HEADS = 4
M_QK_DIM = 128
M_V_DIM = 256
M_QK = M_HEADS * M_QK_DIM
M_V = M_HEADS * M_V_DIM
CONV_W = 4
CHUNK = 128
POOL_WINDOWS = (2, 4, 8, 16)
POOL_GROUPS = 4
POOL_GROUP_DIM = 256
POOL_W = POOL_GROUPS * POOL_GROUP_DIM
IN_SPLITS = (2 * M_QK, M_V, M_V, M_HEADS, M_HEADS, POOL_W, D_MODEL, D_MODEL)
IN_DIM = sum(IN_SPLITS)
IN_OFFSETS = tuple(int(s) for s in np.cumsum(IN_SPLITS)[:-1])
F_GATE_OFF = 2 * M_QK + 2 * M_V + M_HEADS
N_GROUPS = 4
EXPERTS_PER_GROUP = 8
N_EXPERTS = N_GROUPS * EXPERTS_PER_GROUP
TOP_K = 2
D_EXPERT = 768
MOE_BLOCK = 128
ALPHA = (2 * DEPTH) ** 0.25
BETA = (8 * DEPTH) ** -0.25
LN_EPS = 1e-5

kernel_name = 'hybrid_mlstm_pool_hmoe_deepnorm'


def layer_norm(x, g, b):
    xf = x.astype(jnp.float32)
    mu = xf.mean(-1, keepdims=True)
    var = jnp.square(xf - mu).mean(-1, keepdims=True)
    return ((xf - mu) * lax.rsqrt(var + LN_EPS) * g + b).astype(x.dtype)


def causal_dwconv(u, w, b):
    S = u.shape[1]
    up = jnp.pad(u, ((0, 0), (CONV_W - 1, 0), (0, 0)))
    out = b + up[:, 0:S] * w[0]
    for tap in range(1, CONV_W):
        out = out + up[:, tap:tap + S] * w[tap]
    return out


def mlstm_chunkwise(q, k, v, ig, fg):
    B, S, H, _ = q.shape
    nc = S // CHUNK
    f32 = jnp.float32

    def to_chunks(a):
        a = a.astype(f32).reshape((B, nc, CHUNK, H) + a.shape[3:])
        return jnp.moveaxis(a, (1, 3), (0, 2))

    qc = to_chunks(q)
    kc = to_chunks(k) * (M_QK_DIM ** -0.5)
    vc = to_chunks(v)
    lfc = jax.nn.log_sigmoid(to_chunks(fg))
    igc = to_chunks(ig)
    causal = jnp.tril(jnp.ones((CHUNK, CHUNK), bool))

    def step(carry, inp):
        C, n, m = carry
        qb, kb, vb, lf, ib = inp
        b = jnp.cumsum(lf, axis=-1)
        dmat = jnp.where(causal, b[..., :, None] - b[..., None, :] + ib[..., None, :], -jnp.inf)
        inter = b + m[..., None]
        m_row = jnp.maximum(inter, dmat.max(-1))
        s = jnp.einsum('bhjd,bhld->bhjl', qb, kb) * jnp.exp(dmat - m_row[..., None])
        scale_prev = jnp.exp(inter - m_row)
        num = jnp.einsum('bhjl,bhlv->bhjv', s, vb) + scale_prev[..., None] * jnp.einsum('bhvd,bhjd->bhjv', C, qb)
        den = s.sum(-1) + scale_prev * jnp.einsum('bhd,bhjd->bhj', n, qb)
        h = num / jnp.maximum(jnp.abs(den), jnp.exp(-m_row))[..., None]
        b_last = b[..., -1]
        g = b_last[..., None] - b + ib
        m_new = jnp.maximum(b_last + m, g.max(-1))
        w = jnp.exp(g - m_new[..., None])
        decay = jnp.exp(b_last + m - m_new)
        C = decay[..., None, None] * C + jnp.einsum('bhl,bhlv,bhld->bhvd', w, vb, kb)
        n = decay[..., None] * n + jnp.einsum('bhl,bhld->bhd', w, kb)
        return (C, n, m_new), h

    init = (jnp.zeros((B, H, M_V_DIM, M_QK_DIM), f32), jnp.zeros((B, H, M_QK_DIM), f32), jnp.zeros((B, H), f32))
    _, hs = lax.scan(step, init, (qc, kc, vc, lfc, igc))
    return jnp.moveaxis(hs, (0, 2), (1, 3)).reshape(B, S, H, M_V_DIM)


def multiscale_pool(u):
    S = u.shape[1]
    uf = u.astype(jnp.float32)
    cs = jnp.pad(jnp.cumsum(uf, axis=1), ((0, 0), (1, 0), (0, 0)))
    t = jnp.arange(S)
    outs = []
    for g, win in enumerate(POOL_WINDOWS):
        sl = slice(g * POOL_GROUP_DIM, (g + 1) * POOL_GROUP_DIM)
        c = cs[:, :, sl]
        lo = jnp.maximum(t + 1 - win, 0)
        cnt = jnp.minimum(t + 1, win).astype(jnp.float32)
        outs.append((c[:, 1:] - c[:, lo]) / cnt[None, :, None] - uf[:, :, sl])
    return jnp.concatenate(outs, axis=-1).astype(u.dtype)


def token_mixer(x, w_in, b_in, conv_w, conv_b, mh_g, w_pool, pool_scale, w_m_br, w_p_br, w_out):
    B, S, _ = x.shape
    z = x @ w_in + b_in
    qk_pre, v, o, ig, fg, u, gm, gp = jnp.split(z, IN_OFFSETS, axis=-1)
    qk = jax.nn.silu(causal_dwconv(qk_pre, conv_w, conv_b))
    q, k = jnp.split(qk, [M_QK], axis=-1)
    h = mlstm_chunkwise(q.reshape(B, S, M_HEADS, M_QK_DIM), k.reshape(B, S, M_HEADS, M_QK_DIM),
                        v.reshape(B, S, M_HEADS, M_V_DIM), ig, fg)
    mu = h.mean(-1, keepdims=True)
    var = jnp.square(h - mu).mean(-1, keepdims=True)
    h = (h - mu) * lax.rsqrt(var + LN_EPS) * mh_g.reshape(M_HEADS, M_V_DIM)
    h = h.reshape(B, S, M_V).astype(x.dtype) * jax.nn.sigmoid(o)
    a = h @ w_m_br
    y = multiscale_pool(u).reshape(B, S, POOL_GROUPS, POOL_GROUP_DIM)
    y = jnp.einsum('bsgc,gcd->bsgd', y, w_pool).reshape(B, S, POOL_W) * pool_scale
    pb = y @ w_p_br
    merged = jax.nn.sigmoid(gm) * a + jax.nn.sigmoid(gp) * pb
    return merged @ w_out


def hier_moe(x, w_rg, b_rg, w_re, b_re, w_gate, w_up, w_down):
    B, S, D = x.shape
    T = B * S
    xf = x.reshape(T, D)
    pg = jax.nn.softmax((xf @ w_rg + b_rg).astype(jnp.float32), axis=-1)
    pg_top, g_idx = lax.top_k(pg, 1)
    le = (xf @ w_re + b_re).astype(jnp.float32).reshape(T, N_GROUPS, EXPERTS_PER_GROUP)
    le_sel = jnp.take_along_axis(le, g_idx[:, :, None], axis=1)[:, 0]
    pe = jax.nn.softmax(le_sel, axis=-1)
    pe_top, e_local = lax.top_k(pe, TOP_K)
    gate = pg_top * pe_top / pe_top.sum(-1, keepdims=True)
    e_idx = g_idx * EXPERTS_PER_GROUP + e_local
    A = T * TOP_K
    flat_e = e_idx.reshape(A)
    flat_tok = jnp.repeat(jnp.arange(T, dtype=jnp.int32), TOP_K)
    flat_w = gate.reshape(A)
    order = jnp.argsort(flat_e)
    se, stok, sw = flat_e[order], flat_tok[order], flat_w[order]
    counts = jnp.bincount(flat_e, length=N_EXPERTS)
    starts = jnp.cumsum(counts) - counts
    pcounts = (counts + MOE_BLOCK - 1) // MOE_BLOCK * MOE_BLOCK
    pends = jnp.cumsum(pcounts)
    pstarts = pends - pcounts
    dest = pstarts[se] + jnp.arange(A) - starts[se]
    P = A + N_EXPERTS * MOE_BLOCK
    NB = P // MOE_BLOCK
    slot_tok = jnp.zeros((P,), jnp.int32).at[dest].set(stok)
    slot_w = jnp.zeros((P,), jnp.float32).at[dest].set(sw)
    block_e = jnp.minimum(jnp.searchsorted(pends, jnp.arange(NB) * MOE_BLOCK, side='right'), N_EXPERTS - 1)
    xb = xf[slot_tok].reshape(NB, MOE_BLOCK, D)

    def expert_block(args):
        xblk, e = args
        hid = jax.nn.silu(xblk @ w_gate[e]) * (xblk @ w_up[e])
        return hid @ w_down[e]

    yb = lax.map(expert_block, (xb, block_e)).reshape(P, D)
    yb = yb * slot_w[:, None].astype(yb.dtype)
    out = jnp.zeros((T, D), yb.dtype).at[slot_tok].add(yb)
    return out.reshape(B, S, D)


def setup_inputs(seed: int = 0) -> dict:
    key = jax.random.key(seed)
    ks = jax.random.split(key, 32)

    def nrm(k, shape, scale):
        return scale * jax.random.normal(k, shape, jnp.float32)

    sd = D_MODEL ** -0.5
    x = nrm(ks[0], (BATCH, SEQ, D_MODEL), 1.0)
    p = nrm(ks[1], (DEPTH, BATCH, SEQ, PLE_DIM), 1.0)
    w_in = jnp.concatenate([
        nrm(ks[2], (DEPTH, D_MODEL, 2 * M_QK), sd),
        nrm(ks[3], (DEPTH, D_MODEL, M_V), sd * BETA),
        nrm(ks[4], (DEPTH, D_MODEL, M_V), sd),
        nrm(ks[5], (DEPTH, D_MODEL, 2 * M_HEADS), sd),
        nrm(ks[6], (DEPTH, D_MODEL, POOL_W), sd),
        nrm(ks[7], (DEPTH, D_MODEL, 2 * D_MODEL), sd)], axis=-1)
    f_bias = jnp.broadcast_to(jnp.linspace(3.0, 6.0, M_HEADS, dtype=jnp.float32), (DEPTH, M_HEADS))
    b_in = nrm(ks[8], (DEPTH, IN_DIM), 0.02).at[:, F_GATE_OFF:F_GATE_OFF + M_HEADS].add(f_bias)
    conv_w = nrm(ks[9], (DEPTH, CONV_W, 2 * M_QK), CONV_W ** -0.5)
    conv_b = nrm(ks[10], (DEPTH, 2 * M_QK), 0.02)
    mh_g = 1.0 + nrm(ks[11], (DEPTH, M_V), 0.02)
    w_pool = nrm(ks[12], (DEPTH, POOL_GROUPS, POOL_GROUP_DIM, POOL_GROUP_DIM), POOL_GROUP_DIM ** -0.5)
    pool_scale = 1.0 + nrm(ks[13], (DEPTH, POOL_W), 0.1)
    w_m_br = nrm(ks[14], (DEPTH, M_V, D_MODEL), M_V ** -0.5)
    w_p_br = nrm(ks[15], (DEPTH, POOL_W, D_MODEL), POOL_W ** -0.5)
    w_out = nrm(ks[16], (DEPTH, D_MODEL, D_MODEL), sd * BETA)
    ln1_g = 1.0 + nrm(ks[17], (DEPTH, D_MODEL), 0.02)
    ln1_b = nrm(ks[18], (DEPTH, D_MODEL), 0.02)
    w_rg = nrm(ks[19], (DEPTH, D_MODEL, N_GROUPS), sd)
    b_rg = nrm(ks[20], (DEPTH, N_GROUPS), 0.01)
    w_re = nrm(ks[21], (DEPTH, D_MODEL, N_EXPERTS), sd)
    b_re = nrm(ks[22], (DEPTH, N_EXPERTS), 0.01)
    w_gate = nrm(ks[23], (DEPTH, N_EXPERTS, D_MODEL, D_EXPERT), sd)
    w_up = nrm(ks[24], (DEPTH, N_EXPERTS, D_MODEL, D_EXPERT), sd)
    w_down = nrm(ks[25], (DEPTH, N_EXPERTS, D_EXPERT, D_MODEL), (D_EXPERT ** -0.5) * BETA)
    ln2_g = 1.0 + nrm(ks[26], (DEPTH, D_MODEL), 0.02)
    ln2_b = nrm(ks[27], (DEPTH, D_MODEL), 0.02)
    w_ple_gate = nrm(ks[28], (DEPTH, D_MODEL, D_MODEL), sd)
    b_ple_gate = nrm(ks[29], (DEPTH, D_MODEL), 0.02)
    w_ple_proj = nrm(ks[30], (DEPTH, PLE_DIM, D_MODEL), (PLE_DIM ** -0.5) * BETA)
    return {'x': x, 'p': p, 'w_in': w_in, 'b_in': b_in, 'conv_w': conv_w, 'conv_b': conv_b,
            'mh_g': mh_g, 'w_pool': w_pool, 'pool_scale': pool_scale, 'w_m_br': w_m_br,
            'w_p_br': w_p_br, 'w_out': w_out, 'ln1_g': ln1_g, 'ln1_b': ln1_b, 'w_rg': w_rg,
            'b_rg': b_rg, 'w_re': w_re, 'b_re': b_re, 'w_gate': w_gate, 'w_up': w_up,
            'w_down': w_down, 'ln2_g': ln2_g, 'ln2_b': ln2_b, 'w_ple_gate': w_ple_gate,
            'b_ple_gate': b_ple_gate, 'w_ple_proj': w_ple_proj}


def reference(x, p, w_in, b_in, conv_w, conv_b, mh_g, w_pool, pool_scale, w_m_br, w_p_br, w_out,
              ln1_g, ln1_b, w_rg, b_rg, w_re, b_re, w_gate, w_up, w_down, ln2_g, ln2_b,
              w_ple_gate, b_ple_gate, w_ple_proj):
    for i in range(DEPTH):
        h = token_mixer(x, w_in[i], b_in[i], conv_w[i], conv_b[i], mh_g[i], w_pool[i], pool_scale[i],
                        w_m_br[i], w_p_br[i], w_out[i])
        x = layer_norm(ALPHA * x + h, ln1_g[i], ln1_b[i])
        h = hier_moe(x, w_rg[i], b_rg[i], w_re[i], b_re[i], w_gate[i], w_up[i], w_down[i])
        x = layer_norm(ALPHA * x + h, ln2_g[i], ln2_b[i])
        x = x + jax.nn.sigmoid(x @ w_ple_gate[i] + b_ple_gate[i]) * (p[i] @ w_ple_proj[i])
    return x
```

```python
import contextlib
import numpy as np
import concourse.bass as bass
import concourse.mybir as mybir
from concourse.bass_utils import run_bass_kernel_spmd

F32 = mybir.dt.float32
BF16 = mybir.dt.bfloat16
I32 = mybir.dt.int32
U32 = mybir.dt.uint32
AF = mybir.ActivationFunctionType
ALU = mybir.AluOpType
AX = mybir.AxisListType

PE, ACT, DVE, POOL, SP = "tensor", "scalar", "vector", "gpsimd", "sync"
COMPUTE = (PE, ACT, DVE, POOL)
ALL_ENG = (PE, ACT, DVE, POOL, SP)

D = 2048
KC = 16
TB = 512
NTT = 4
NBLK = 4
NPRE = 4
TOK = 2048
NH = 4
DV = 256
DVA = 257
CAP = 256
NE = 32
DE = 768
ALPHA = 2.0 ** 0.25
LN_EPS = 1e-5
LNS = float(np.log(128.0 ** -0.5))
WINS = (2, 4, 8, 16)
OFF_Q, OFF_K, OFF_V, OFF_O, OFF_U, OFF_GM, OFF_GP = 0, 512, 1024, 2048, 3080, 4104, 6152


class Buf:
    __slots__ = ("name", "w", "r", "pr")

    def __init__(self, name=""):
        self.name = name
        self.w = []
        self.r = []
        self.pr = []


class Op:
    __slots__ = ("eng", "fn", "deps", "is_dma", "need_sig", "sem", "val", "dma_prev")

    def __init__(self, eng, fn, is_dma):
        self.eng = eng
        self.fn = fn
        self.deps = []
        self.is_dma = is_dma
        self.need_sig = False
        self.sem = None
        self.val = None
        self.dma_prev = None


class Sched:
    def __init__(self, nc, n_dma_sems=48):
        self.nc = nc
        self.ops = {e: [] for e in ALL_ENG}
        self.n_dma_sems = n_dma_sems
        self.all_ops = []
        self.last = {e: None for e in ALL_ENG}
        self.dmas_since_barrier = []
        self.pending_barrier = {e: None for e in ALL_ENG}

    def op(self, eng, fn, reads=(), writes=(), dma=False, acc=False, extra_deps=(), ring=False):
        o = Op(eng, fn, dma)
        deps = []
        for b in reads:
            deps.extend(b.w)
        for b in writes:
            deps.extend(b.r)
            if acc and not b.r:
                deps.extend(b.pr)
            if not acc:
                deps.extend(b.w)
        deps.extend(extra_deps)
        if self.pending_barrier[eng] is not None:
            deps.append(self.pending_barrier[eng])
            self.pending_barrier[eng] = None
        seen = set()
        for d in deps:
            if d is o or id(d) in seen:
                continue
            seen.add(id(d))
            o.deps.append(d)
        for b in reads:
            b.r.append(o)
        for b in writes:
            if b.r or not acc:
                if b.r:
                    b.pr = b.r
                b.w = [o]
                b.r = []
            else:
                b.w.append(o)
        self.ops[eng].append(o)
        self.all_ops.append(o)
        self.last[eng] = o
        if dma and not ring:
            self.dmas_since_barrier.append(o)
        return o

    def barrier(self, fn, skip=()):
        deps = [self.last[e] for e in COMPUTE if self.last[e] is not None and e not in skip]
        deps += self.dmas_since_barrier
        self.dmas_since_barrier = []
        o = self.op(DVE, fn, extra_deps=deps)
        for e in ALL_ENG:
            if e != DVE and e not in skip:
                self.pending_barrier[e] = o
        return o

    def emit(self, final_waits=()):
        nc = self.nc
        for o in self.all_ops:
            kept = []
            for d in o.deps:
                if (not d.is_dma) and (not o.is_dma) and d.eng == o.eng and o.eng == PE:
                    continue
                kept.append(d)
            o.deps = kept
            for d in kept:
                d.need_sig = True
        for o in final_waits:
            o.need_sig = True
        with contextlib.ExitStack() as st:
            eng_sem = {e: st.enter_context(nc.semaphore(f"s_{e}")) for e in COMPUTE}
            cnt = {e: 0 for e in COMPUTE}
            dma_sems = [st.enter_context(nc.semaphore(f"s_dma_{i}")) for i in range(self.n_dma_sems)]
            dma_cnt = [0] * self.n_dma_sems
            dma_last = [None] * self.n_dma_sems
            di = 0
            for o in self.all_ops:
                if o.is_dma:
                    k = di % self.n_dma_sems
                    di += 1
                    o.dma_prev = dma_last[k]
                    dma_cnt[k] += 16
                    o.sem, o.val = dma_sems[k], dma_cnt[k]
                    dma_last[k] = o
                    o.need_sig = True
                elif o.need_sig:
                    cnt[o.eng] += 1
                    o.sem, o.val = eng_sem[o.eng], cnt[o.eng]
            blk = st.enter_context(nc.Block())

            def make(eng_name):
                ops = self.ops[eng_name]

                def body(eng):
                    waited = {}

                    def wait(d):
                        key = id(d.sem)
                        if waited.get(key, 0) >= d.val:
                            return
                        eng.wait_ge(d.sem, d.val)
                        waited[key] = d.val
                    for o in ops:
                        for d in o.deps:
                            wait(d)
                        if o.is_dma and o.dma_prev is not None:
                            wait(o.dma_prev)
                        ins = o.fn(eng)
                        if o.need_sig:
                            ins.then_inc(o.sem, 16 if o.is_dma else 1)
                    if eng_name == SP:
                        for o in final_waits:
                            wait(o)
                return body
            blk.tensor(make(PE))
            blk.scalar(make(ACT))
            blk.vector(make(DVE))
            blk.gpsimd(make(POOL))
            blk.sync(make(SP))


class T:
    __slots__ = ("ap", "buf")

    def __init__(self, ap, name=""):
        self.ap = ap
        self.buf = Buf(name)

    def __getitem__(self, k):
        return self.ap[k]


class Arena:
    def __init__(self, ap_f32, nbytes):
        self.base = ap_f32
        self.nbytes = nbytes
        self.off = 0
        self.marks = []

    def alloc(self, shape, dt, name=""):
        esz = 2 if dt == BF16 else 4
        n = 1
        for x in shape[1:]:
            n *= x
        nb = (n * esz + 31) // 32 * 32
        assert self.off + nb <= self.nbytes, f"arena overflow allocating {name}: {self.off}+{nb}>{self.nbytes}"
        v = self.base[0:shape[0], self.off // 4:(self.off + nb) // 4]
        self.off += nb
        if dt != F32:
            v = v.bitcast(dt)
        v = v[:, 0:n]
        if len(shape) == 3:
            v = v.rearrange("p (a b) -> p a b", a=shape[1])
        elif len(shape) == 4:
            v = v.rearrange("p (a b c) -> p a b c", a=shape[1], b=shape[2])
        return T(v, name)

    def mark(self):
        return self.off

    def reset(self, m):
        self.off = m


def build_program(debug=False):
    nc = bass.Bass("TRN2", target_bir_lowering=False)

    def din(name, shape, dt=F32):
        return nc.dram_tensor(name, list(shape), dt, kind="ExternalInput").ap()

    xT_d = din("xT", [D, 2 * TOK])
    xtok_d = din("xtok", [TOK, D])
    pT_d = din("pT", [256, TOK])
    flag_d = din("flag", [128, 1])
    cmat_d = din("cmat", [128, 4, 128])
    w_in_d = din("w_in", [D, 8200])
    w_if_d = din("w_if", [D, 64])
    bfm_d = din("b_fm", [128, 56])
    btm_d = din("b_tm", [128, 2048])
    convw_d = din("convw", [128, 8, 5])
    mhg_d = din("mhg", [128, 1024])
    poolsc_d = din("pool_sc", [128, 8])
    wpool_d = din("w_pool", [4, 256, 256])
    wm_d = din("w_m_br", [1024, D])
    wp_d = din("w_p_br", [1024, D])
    wout_d = din("w_out", [D, D])
    ln_d = din("ln", [128, 4, D])
    wr_d = din("w_r", [D, 36])
    br_d = din("b_r", [128, 36])
    wg_d = din("w_gate", [NE, D, DE])
    wu_d = din("w_up", [NE, D, DE])
    wd_d = din("w_down", [NE, DE, D])
    wpg_d = din("w_ple_gate", [D, D])
    bpg_d = din("b_pg", [128, D])
    wpp_d = din("w_ple_proj", [256, D])
    out_d = nc.dram_tensor("out", [TOK, D], F32, kind="ExternalOutput").ap()
    XE_d = nc.dram_tensor("XE", [NE * CAP, D], BF16, kind="Internal").ap()
    YE_d = nc.dram_tensor("YE", [NE * CAP, D], F32, kind="Internal").ap()
    X1_d = nc.dram_tensor("X1", [TOK, D], F32, kind="Internal").ap()
    dbg = {}
    if debug:
        dbg["x1"] = nc.dram_tensor("dbg_x1", [TOK, D], F32, kind="ExternalOutput").ap()
        dbg["h"] = nc.dram_tensor("dbg_h", [TOK, 1024], F32, kind="ExternalOutput").ap()
        dbg["mrg"] = nc.dram_tensor("dbg_mrg", [D, TOK], F32, kind="ExternalOutput").ap()
        dbg["rt"] = nc.dram_tensor("dbg_rt", [TOK, 4], F32, kind="ExternalOutput").ap()
        dbg["moe"] = nc.dram_tensor("dbg_moe", [TOK, D], F32, kind="ExternalOutput").ap()
        dbg["res"] = nc.dram_tensor("dbg_res", [TOK, D], F32, kind="ExternalOutput").ap()

    ARENA_BYTES = 204800
    with contextlib.ExitStack() as st:
        arena_t = st.enter_context(nc.sbuf_tensor("arena", [128, ARENA_BYTES // 4], F32))
        ar = Arena(arena_t, ARENA_BYTES)
        psum_t = [st.enter_context(nc.psum_tensor(f"ps{i}", [128, 512], F32)) for i in range(8)]
        banks = [T(psum_t[i][:, :], f"bank{i}") for i in range(8)]
        s = Sched(nc)
        state = {"pi": 0, "pool": list(range(8))}

        def psum():
            pool = state["pool"]
            b = banks[pool[state["pi"] % len(pool)]]
            state["pi"] += 1
            return b

        def op(eng, fn, reads=(), writes=(), **kw):
            return s.op(eng, fn, reads=[t.buf for t in reads], writes=[t.buf for t in writes], **kw)

        def dma(eng, out_ap, in_ap, reads=(), writes=(), acc=True, ring=False):
            return op(eng, lambda e: e.dma_start(out=out_ap, in_=in_ap), reads=reads, writes=writes, dma=True, acc=acc, ring=ring)

        def act(out_ap, in_ap, func, reads, writes, bias=None, scale=None, **kw):
            def f(e):
                kws = {}
                if bias is not None:
                    kws["bias"] = bias
                if scale is not None:
                    kws["scale"] = scale
                return e.activation(out=out_ap, in_=in_ap, func=func, **kws)
            return op(ACT, f, reads=reads, writes=writes, **kw)

        def tt(out_ap, in0, in1, alu, reads, writes, eng=DVE):
            return op(eng, lambda e: e.tensor_tensor(out=out_ap, in0=in0, in1=in1, op=alu), reads=reads, writes=writes)

        def ts(out_ap, in0, s1, s2, op0, op1, reads, writes, eng=DVE):
            if s2 is None:
                return op(eng, lambda e: e.tensor_scalar(out=out_ap, in0=in0, scalar1=s1, scalar2=None, op0=op0), reads=reads, writes=writes)
            return op(eng, lambda e: e.tensor_scalar(out=out_ap, in0=in0, scalar1=s1, scalar2=s2, op0=op0, op1=op1), reads=reads, writes=writes)

        def stt(out_ap, in0, sc, in1, op0, op1, reads, writes):
            return op(DVE, lambda e: e.scalar_tensor_tensor(out=out_ap, in0=in0, scalar=sc, in1=in1, op0=op0, op1=op1), reads=reads, writes=writes)

        def cp(out_ap, in_ap, reads, writes, eng=DVE):
            return op(eng, lambda e: e.tensor_copy(out=out_ap, in_=in_ap), reads=reads, writes=writes)

        def mm(out_ap, lhsT, rhs, start, stop, reads, writes):
            return op(PE, lambda e: e.matmul(out_ap, lhsT=lhsT, rhs=rhs, start=start, stop=stop), reads=reads, writes=writes, acc=True)

        def tr(out_ap, in_ap, ident, reads, writes):
            return op(PE, lambda e: e.transpose(out=out_ap, in_=in_ap, identity=ident), reads=reads, writes=writes, acc=True)

        def memset(ap, val, writes, eng=DVE):
            return op(eng, lambda e: e.memset(ap, val), writes=writes)

        cm_bf = ar.alloc([128, 4, 128], BF16, "cm_bf")
        cm_f = ar.alloc([128, 4, 128], F32, "cm_f")
        flag = ar.alloc([128, 1], F32, "flag")
        slots = ar.alloc([128, 16, 2], I32, "slots")
        gates = ar.alloc([128, 16, 2], F32, "gates")
        acum = ar.alloc([128, 32], BF16, "acum")
        dummy = ar.alloc([128, 8], F32, "dummy")
        dma(SP, cm_f[:], cmat_d, writes=[cm_f])
        dma(POOL, cm_bf[:], cmat_d, writes=[cm_bf])
        dma(SP, flag[:], flag_d, writes=[flag])
        memset(acum[:], 0.0, [acum])
        ident_bf = cm_bf[:, 0, :]
        mask_bf = cm_bf[:, 1, :]
        lstr_bf = cm_bf[:, 2, :]
        ones_bf = cm_bf[:, 3, :]
        ident4 = cm_f[0:4, 0, 0:4]
        ones4 = cm_f[0:4, 3, :]
        g_mark = ar.mark()

        def barrier(skip=()):
            s.barrier(lambda e: e.memset(dummy[:, 0:1], 0.0), skip=skip)

        bfm = ar.alloc([128, 56], F32, "bfm")
        btm = ar.alloc([128, 2048], F32, "btm")
        convw = ar.alloc([128, 8, 5], F32, "convw")
        mhg = ar.alloc([128, 1024], F32, "mhg")
        poolsc = ar.alloc([128, 8], F32, "poolsc")
        wpool = ar.alloc([128, 4, 2, 256], BF16, "wpool")
        wr = ar.alloc([128, KC, 36], BF16, "wr")
        brt = ar.alloc([128, 36], F32, "brt")
        Cst = [ar.alloc([128, DVA], F32, f"Cst{h}") for h in range(NH)]
        Cs = [ar.alloc([128, DVA], F32, f"Cs{h}") for h in range(NH)]
        Csb = [ar.alloc([128, DVA], BF16, f"Csb{h}") for h in range(NH)]
        mst = ar.alloc([4, 1], F32, "mst")
        qkcar = ar.alloc([128, 8, 3], F32, "qkcar")
        ucar = ar.alloc([128, 8, 15], F32, "ucar")
        invc = ar.alloc([128, 4, 16], F32, "invc")
        negbf = ar.alloc([4, 1], F32, "negbf")
        ones_row = ar.alloc([4, TB], F32, "ones_row")
        dma(SP, bfm[:], bfm_d, writes=[bfm])
        dma(SP, btm[:], btm_d, writes=[btm])
        dma(SP, convw[:], convw_d, writes=[convw])
        dma(SP, mhg[:], mhg_d, writes=[mhg])
        dma(SP, poolsc[:], poolsc_d, writes=[poolsc])
        for g in range(4):
            dma(POOL, wpool[:, g, :, :], wpool_d[g].rearrange("(kc p) n -> p kc n", p=128), writes=[wpool])
        dma(POOL, wr[:], wr_d.rearrange("(kc p) n -> p kc n", p=128), writes=[wr])
        dma(SP, brt[:], br_d, writes=[brt])
        for h in range(NH):
            memset(Cst[h][:], 0.0, [Cst[h]])
        memset(mst[:], 0.0, [mst])
        memset(qkcar[:], 0.0, [qkcar])
        memset(ucar[:], 0.0, [ucar])
        memset(ones_row[:], 1.0, [ones_row])
        ts(negbf[:], bfm[0:4, 49:50], -1.0, None, ALU.mult, None, [bfm], [negbf])
        f2k = ar.alloc([128, 1], F32, "f2k")
        ts(f2k[:], flag[:], 2048.0, None, ALU.mult, None, [flag], [f2k])
        iot = ar.alloc([128, 16], F32, "iot")
        op(POOL, lambda e: e.iota(iot[:], pattern=[[1, 16]], base=1, channel_multiplier=0, allow_small_or_imprecise_dtypes=True), writes=[iot])
        for g in range(4):
            ts(invc[:, g, :], iot[:], f2k[:, 0:1], float(WINS[g]), ALU.add, ALU.min, [iot, f2k], [invc])
        op(DVE, lambda e: e.reciprocal(out=invc[:], in_=invc[:]), reads=[invc], writes=[invc])

        NW = 3
        wbuf = [ar.alloc([128, KC, 512], BF16, f"wbuf{i}") for i in range(NW)]
        wstate = {"i": 0}
        xT = ar.alloc([128, KC, TB], BF16, "xT")
        hT = ar.alloc([128, 8, TB], BF16, "hT")
        y2T = ar.alloc([128, 8, TB], BF16, "y2T")
        mrgT = ar.alloc([128, KC, TB], BF16, "mrgT")
        a_mark = ar.mark()

        def next_wbuf():
            w = wbuf[wstate["i"] % NW]
            wstate["i"] += 1
            return w

        def load_cg(src_ap_fn, nk=KC, ncols=512):
            w = next_wbuf()
            for k0 in range(0, nk, 4):
                dma(POOL, w[:, k0:k0 + 4, 0:ncols], src_ap_fn(k0, 4), writes=[w], ring=True)
            return w

        def win_cols(c0, ncols=512):
            v = w_in_d.rearrange("(kc p) n -> p kc n", p=128)
            return lambda k0, n: v[:, k0:k0 + n, c0:c0 + ncols]

        def load_xT(tok0):
            v = xT_d.rearrange("(kc p) n -> p kc n", p=128)
            for k0 in range(0, KC, 4):
                dma(POOL, xT[:, k0:k0 + 4, :], v[:, k0:k0 + 4, tok0:tok0 + TB], writes=[xT], ring=True)

        def fm_group(w, ncc, consume):
            for cc in range(ncc):
                ps = psum()
                for k in range(KC):
                    mm(ps[:, :], w[:, k, cc * 128:(cc + 1) * 128], xT[:, k, :], k == 0, k == KC - 1, [w, xT], [ps])
                consume(cc, ps)

        def tm_group(w, consume, lhs=None):
            lhs = xT if lhs is None else lhs
            for t4 in range(NTT):
                ps = psum()
                for k in range(KC):
                    mm(ps[:, :], lhs[:, k, t4 * 128:(t4 + 1) * 128], w[:, k, :], k == 0, k == KC - 1, [w, lhs], [ps])
                consume(t4, ps)

        def block(tok0, kind, blk_idx):
            main = kind == "main"
            ar.reset(a_mark)
            gi = ar.alloc([4, TB], F32, "gi")
            bneg = ar.alloc([4, TB], F32, "bneg")
            aa = ar.alloc([4, TB], F32, "aa")
            arg1 = ar.alloc([4, TB], F32, "arg1")
            arg2 = ar.alloc([4, TB], F32, "arg2")
            small = ar.alloc([4, 32], F32, "small")
            EX = ar.alloc([128, NTT, 12], F32, "EX")
            pre = [ar.alloc([128, TB + 3], F32, f"pre{i}") for i in range(2)]
            cacc = [ar.alloc([128, TB], F32, f"cacc{i}") for i in range(2)]
            qkT = ar.alloc([128, 8, TB], BF16, "qkT")
            kp = [ar.alloc([128, 128], BF16, f"kp{i}") for i in range(4)]
            vaug = ar.alloc([128, NTT, NH, DVA], BF16, "vaug")
            load_xT(tok0)
            memset(vaug[:, :, :, 256:257], 1.0, [vaug])
            if main:
                gso = ar.alloc([128, NTT, 1024], BF16, "gso")
                otmp = [ar.alloc([128, 512], F32, f"otmp{i}") for i in range(2)]
                S_sb = [ar.alloc([128, 128], BF16, f"S{i}") for i in range(4)]
                htok = [ar.alloc([128, 1024], BF16, f"htok{i}") for i in range(2)]
                hnrm = [ar.alloc([128, 256], F32, f"hnrm{i}") for i in range(2)]
                stat = ar.alloc([128, NH, 6], F32, "stat")
                mv = ar.alloc([128, NH, 2], F32, "mv")
                sm = ar.alloc([128, 8, 4], F32, "sm")
            if kind != "pre":
                ubuf = [ar.alloc([128, TB + 15], F32, f"ubuf{i}") for i in range(2)]
                uw = [ar.alloc([128, TB + 15], F32, f"uw{i}") for i in range(2)]
                yT = [ar.alloc([128, 2, TB], BF16, f"yT{i}") for i in range(2)]

            w = load_cg(lambda k0, n: w_if_d.rearrange("(kc p) n -> p kc n", p=128)[:, k0:k0 + n, :], ncols=64)
            psI, psF = psum(), psum()
            for k in range(KC):
                mm(psI[0:4, :], w[:, k, 0:4], xT[:, k, :], k == 0, k == KC - 1, [w, xT], [psI])
            for k in range(KC):
                mm(psF[0:4, :], w[:, k, 32:36], xT[:, k, :], k == 0, k == KC - 1, [w, xT], [psF])
            act(arg1[:], psF[0:4, :], AF.Exp, [psF, negbf], [arg1], bias=negbf[:, 0:1], scale=-1.0)
            act(arg2[:], arg1[:], AF.Ln, [arg1], [arg2], bias=1.0)
            act(gi[:], psI[0:4, :], AF.Identity, [psI, bfm], [gi], bias=bfm[0:4, 48:49])
            for c in range(NTT):
                cs_ = slice(c * 128, (c + 1) * 128)
                op(DVE, lambda e, cs_=cs_: e.tensor_tensor_scan(out=bneg[:, cs_], data0=ones_row[:, cs_], data1=arg2[:, cs_], initial=0.0, op0=ALU.mult, op1=ALU.add),
                   reads=[ones_row, arg2], writes=[bneg], acc=True)
            tt(aa[:], gi[:], bneg[:], ALU.add, [gi, bneg], [aa])
            op(DVE, lambda e: e.tensor_reduce(out=small[:, 0:4], in_=aa[:].rearrange("p (c l) -> p c l", c=NTT), axis=AX.X, op=ALU.max), reads=[aa], writes=[small])
            for c in range(NTT):
                tt(small[:, 4 + c:5 + c], small[:, c:c + 1], mst[:], ALU.max, [small, mst], [small])
                tt(small[:, 8 + c:9 + c], mst[:], small[:, 4 + c:5 + c], ALU.subtract, [small, mst], [small])
                tt(mst[:], small[:, 4 + c:5 + c], bneg[:, c * 128 + 127:c * 128 + 128], ALU.subtract, [small, bneg], [mst])
                cs_ = slice(c * 128, (c + 1) * 128)
                ts(arg1[:, cs_], aa[:, cs_], small[:, 4 + c:5 + c], LNS, ALU.subtract, ALU.add, [aa, small], [arg1])
                ts(arg2[:, cs_], bneg[:, cs_], small[:, 4 + c:5 + c], None, ALU.subtract, None, [bneg, small], [arg2])
                ts(small[:, 12 + 4 * c:16 + 4 * c], ident4, small[:, 8 + c:9 + c], None, ALU.mult, None, [cm_f, small], [small])
            for c in range(NTT):
                cs_ = slice(c * 128, (c + 1) * 128)
                ps = psum()
                mm(ps[:, 0:4], arg1[:, cs_], ident4, True, True, [arg1, cm_f], [ps])
                mm(ps[:, 4:8], arg2[:, cs_], ident4, True, True, [arg2, cm_f], [ps])
                mm(ps[:, 8:12], ones4, small[:, 12 + 4 * c:16 + 4 * c], True, True, [small, cm_f], [ps])
                act(EX[:, c, :], ps[:, 0:12], AF.Exp, [ps], [EX])

            def conv_chunk(j, ps, full):
                p_ = pre[j % 2]
                a_ = cacc[j % 2]
                act(p_[:, 3:TB + 3], ps[:, :], AF.Identity, [ps, bfm], [p_], bias=bfm[:, j:j + 1])
                cp(p_[:, 0:3], qkcar[:, j, :], [qkcar], [p_])
                if full:
                    ts(a_[:], p_[:, 0:TB], convw[:, j, 0:1], convw[:, j, 4:5], ALU.mult, ALU.add, [p_, convw], [a_])
                    for tap in range(1, 4):
                        stt(a_[:], p_[:, tap:tap + TB], convw[:, j, tap:tap + 1], a_[:], ALU.mult, ALU.add, [p_, convw, a_], [a_])
                    act(qkT[:, j, :], a_[:], AF.Silu, [a_], [qkT])
                cp(qkcar[:, j, :], p_[:, TB:TB + 3], [p_], [qkcar])

            if kind != "pre":
                w = load_cg(win_cols(OFF_Q))
                fm_group(w, 4, lambda cc, ps: conv_chunk(cc, ps, main))
            w = load_cg(win_cols(OFF_K))
            fm_group(w, 4, lambda cc, ps: conv_chunk(4 + cc, ps, True))

            for cg in range(2):
                w = load_cg(win_cols(OFF_V + cg * 512))

                def vcons(t4, ps, cg=cg):
                    tt(vaug[:, t4, 2 * cg:2 * cg + 2, 0:256], ps[:, :].rearrange("p (h c) -> p h c", h=2),
                       btm[:, cg * 512:(cg + 1) * 512].rearrange("p (h c) -> p h c", h=2), ALU.add, [ps, btm], [vaug])
                tm_group(w, vcons)

            if main:
                for cg in range(2):
                    w = load_cg(win_cols(OFF_O + cg * 512))

                    def ocons(t4, ps, cg=cg):
                        o_ = otmp[t4 % 2]
                        tt(o_[:], ps[:, :], btm[:, 1024 + cg * 512:1024 + (cg + 1) * 512], ALU.add, [ps, btm], [o_])
                        act(o_[:], o_[:], AF.Sigmoid, [o_], [o_])
                        tt(gso[:, t4, cg * 512:(cg + 1) * 512], o_[:], mhg[:, cg * 512:(cg + 1) * 512], ALU.mult, [o_, mhg], [gso])
                    tm_group(w, ocons)

            if kind != "pre":
                for cg in range(2):
                    w = load_cg(win_cols(OFF_U + cg * 512))

                    def ucons(cc, ps, cg=cg):
                        j = cg * 4 + cc
                        g = j // 2
                        u_ = ubuf[j % 2]
                        act(u_[:, 15:TB + 15], ps[:, :], AF.Identity, [ps, bfm], [u_], bias=bfm[:, 8 + j:9 + j])
                        cp(u_[:, 0:15], ucar[:, j, :], [ucar], [u_])
                        if main:
                            src = u_
                            sh = 1
                            for stp in range(g + 1):
                                dst = uw[stp % 2]
                                lo = 2 * sh - 1
                                tt(dst[:, lo:TB + 15], src[:, lo:TB + 15], src[:, lo - sh:TB + 15 - sh], ALU.add, [src], [dst])
                                src = dst
                                sh *= 2
                            y_ = yT[g % 2]
                            stt(y_[:, j % 2, :], src[:, 15:TB + 15], 1.0 / WINS[g], u_[:, 15:TB + 15], ALU.mult, ALU.subtract, [src, u_], [y_])
                            if blk_idx == 0:
                                tmpw = uw[(g + 1) % 2]
                                tt(tmpw[:, 0:16], src[:, 15:31], invc[:, g, :], ALU.mult, [src, invc], [tmpw])
                                tt(y_[:, j % 2, 0:16], tmpw[:, 0:16], u_[:, 15:31], ALU.subtract, [tmpw, u_], [y_])
                        cp(ucar[:, j, :], u_[:, TB:TB + 15], [u_], [ucar])
                        if main and j % 2 == 1:
                            y_ = yT[g % 2]
                            for dd in range(2):
                                ps2 = psum()
                                for kc in range(2):
                                    mm(ps2[:, :], wpool[:, g, kc, dd * 128:(dd + 1) * 128], y_[:, kc, :], kc == 0, kc == 1, [wpool, y_], [ps2])
                                act(y2T[:, 2 * g + dd, :], ps2[:, :], AF.Copy, [ps2, poolsc], [y2T], scale=poolsc[:, 2 * g + dd:2 * g + dd + 1])
                    fm_group(w, 4, ucons)

            state["pool"] = [0, 1, 2, 3]
            for c in range(NTT):
                cs_ = slice(c * 128, (c + 1) * 128)
                out_ps = []
                for h in range(NH):
                    kT_h = qkT[:, 4 + h, cs_]
                    qT_h = qkT[:, h, cs_]
                    sk = EX[:, c, h:h + 1]
                    dec = EX[:, c, 8 + h:9 + h]
                    ts(Cs[h][:], Cst[h][:], dec, None, ALU.mult, None, [Cst[h], EX], [Cs[h]])
                    if main:
                        act(Csb[h][:], Cs[h][:], AF.Copy, [Cs[h]], [Csb[h]])
                        ps_s = psum()
                        mm(ps_s[:, 0:128], kT_h, qT_h, True, True, [qkT], [ps_s])
                        S_ = S_sb[h]
                        stt(S_[:], ps_s[:, 0:128], sk, mask_bf, ALU.mult, ALU.mult, [ps_s, EX, cm_bf], [S_])
                    ps_k = psum()
                    kv = ps_k[:, 0:64].bitcast(BF16)
                    tr(kv, kT_h, ident_bf, [qkT, cm_bf], [ps_k])
                    kp_ = kp[h]
                    act(kp_[:], kv, AF.Copy, [ps_k, EX], [kp_], scale=sk)
                    if main:
                        ps_o = banks[4 + h]
                        mm(ps_o[:, 0:DVA], S_[:], vaug[:, c, h, :], True, False, [S_, vaug], [ps_o])
                        mm(ps_o[:, 0:DVA], qT_h, Csb[h][:], False, True, [qkT, Csb[h]], [ps_o])
                        out_ps.append(ps_o)
                    ps_u = psum()
                    mm(ps_u[:, 0:DVA], kp_[:], vaug[:, c, h, :], True, True, [kp_, vaug], [ps_u])
                    tt(Cst[h][:], Cs[h][:], ps_u[:, 0:DVA], ALU.add, [Cs[h], ps_u], [Cst[h]])
                if main:
                    for h in range(NH):
                        op(DVE, lambda e, h=h, p=out_ps[h]: e.bn_stats(out=stat[:, h, :], in_=p[:, 0:256]), reads=[out_ps[h]], writes=[stat])
                        op(DVE, lambda e, h=h: e.bn_aggr(out=mv[:, h, :], in_=stat[:, h, :]), reads=[stat], writes=[mv])
                        act(sm[:, 0, h:h + 1], out_ps[h][:, 256:257], AF.Abs, [out_ps[h]], [sm])
                    tt(sm[:, 0, :], sm[:, 0, :], EX[:, c, 4:8], ALU.max, [sm, EX], [sm])
                    tt(sm[:, 1, :], sm[:, 0, :], sm[:, 0, :], ALU.mult, [sm], [sm])
                    stt(sm[:, 2, :], sm[:, 1, :], LN_EPS, mv[:, :, 1], ALU.mult, ALU.add, [sm, mv], [sm])
                    act(sm[:, 3, :], sm[:, 2, :], AF.Sqrt, [sm], [sm])
                    op(DVE, lambda e: e.reciprocal(out=sm[:, 4, :], in_=sm[:, 3, :]), reads=[sm], writes=[sm])
                    stt(sm[:, 5, :], mv[:, :, 0], -1.0, sm[:, 4, :], ALU.mult, ALU.mult, [sm, mv], [sm])
                    ht_ = htok[c % 2]
                    for h in range(NH):
                        hn = hnrm[h % 2]
                        act(hn[:], out_ps[h][:, 0:256], AF.Identity, [out_ps[h], sm], [hn], bias=sm[:, 5, h:h + 1], scale=sm[:, 4, h:h + 1])
                        tt(ht_[:, h * 256:(h + 1) * 256], hn[:], gso[:, c, h * 256:(h + 1) * 256], ALU.mult, [hn, gso], [ht_])
                    if debug:
                        for h in range(NH):
                            hn = hnrm[h % 2]
                            cp(hn[:], ht_[:, h * 256:(h + 1) * 256], [ht_], [hn])
                            dma(SP, dbg["h"][tok0 - TOK + c * 128:tok0 - TOK + (c + 1) * 128, h * 256:(h + 1) * 256], hn[:], reads=[hn])
                    ps_t = psum()
                    for kc in range(8):
                        tr(ps_t[:, kc * 64:(kc + 1) * 64].bitcast(BF16), ht_[:, kc * 128:(kc + 1) * 128], ident_bf, [ht_, cm_bf], [ps_t])
                    cp(hT[:, :, cs_], ps_t[:, :].bitcast(BF16).rearrange("p (k t) -> p k t", k=8), [ps_t], [hT])
            state["pool"] = list(range(8))
            if not main:
                barrier(skip=(POOL,))
                return

            barrier(skip=(POOL,))
            ar.reset(a_mark)
            sg = [ar.alloc([128, 2, 4, TB], BF16, f"sg{i}") for i in range(2)]
            m1 = [ar.alloc([128, TB], F32, f"m1_{i}") for i in range(2)]
            m2 = [ar.alloc([128, TB], F32, f"m2_{i}") for i in range(2)]
            for dq in range(4):
                sg_ = sg[dq % 2]
                for gi_, off, bcol in ((0, OFF_GM, 16), (1, OFF_GP, 32)):
                    w = load_cg(win_cols(off + dq * 512))

                    def gcons(cc, ps, gi_=gi_, bcol=bcol, dq=dq, sg_=sg_):
                        act(sg_[:, gi_, cc, :], ps[:, :], AF.Sigmoid, [ps, bfm], [sg_], bias=bfm[:, bcol + dq * 4 + cc:bcol + dq * 4 + cc + 1])
                    fm_group(w, 4, gcons)
                w = next_wbuf()
                wmv = wm_d.rearrange("(kc p) n -> p kc n", p=128)
                wpv = wp_d.rearrange("(kc p) n -> p kc n", p=128)
                for k0 in range(0, 8, 4):
                    dma(POOL, w[:, k0:k0 + 4, :], wmv[:, k0:k0 + 4, dq * 512:(dq + 1) * 512], writes=[w], ring=True)
                for k0 in range(0, 8, 4):
                    dma(POOL, w[:, 8 + k0:8 + k0 + 4, :], wpv[:, k0:k0 + 4, dq * 512:(dq + 1) * 512], writes=[w], ring=True)
                for cc in range(4):
                    psa, psp = psum(), psum()
                    for kc in range(8):
                        mm(psa[:, :], w[:, kc, cc * 128:(cc + 1) * 128], hT[:, kc, :], kc == 0, kc == 7, [w, hT], [psa])
                    for kc in range(8):
                        mm(psp[:, :], w[:, 8 + kc, cc * 128:(cc + 1) * 128], y2T[:, kc, :], kc == 0, kc == 7, [w, y2T], [psp])
                    a1, a2 = m1[cc % 2], m2[cc % 2]
                    tt(a1[:], psa[:, :], sg_[:, 0, cc, :], ALU.mult, [psa, sg_], [a1])
                    tt(a2[:], psp[:, :], sg_[:, 1, cc, :], ALU.mult, [psp, sg_], [a2])
                    tt(mrgT[:, dq * 4 + cc, :], a1[:], a2[:], ALU.add, [a1, a2], [mrgT])
                    if debug:
                        tt(a1[:], a1[:], a2[:], ALU.add, [a1, a2], [a1])
                        dma(SP, dbg["mrg"][(dq * 4 + cc) * 128:(dq * 4 + cc + 1) * 128, tok0 - TOK:tok0 - TOK + TB], a1[:], reads=[a1])

            barrier(skip=(POOL,))
            ar.reset(a_mark)
            ln1 = ar.alloc([128, 2, D], F32, "ln1")
            for i in range(2):
                dma(SP, ln1[:, i, :], ln_d[:, i, :], writes=[ln1])
            res = [ar.alloc([128, D], F32, f"res{i}") for i in range(NTT)]
            x1b = ar.alloc([128, D], BF16, "x1b")
            x1T = ar.alloc([128, KC, 128], BF16, "x1T")
            lst = ar.alloc([128, 4, 6], F32, "lst")
            lmv = ar.alloc([128, 8], F32, "lmv")
            rt = ar.alloc([128, 176], F32, "rt")
            rti = ar.alloc([128, 8], U32, "rti")
            abf = ar.alloc([128, 32], BF16, "abf")
            t0 = tok0 - TOK
            for t4 in range(NTT):
                for hh in range(2):
                    dma(SP, res[t4][:, hh * 1024:(hh + 1) * 1024], xtok_d[t0 + t4 * 128:t0 + (t4 + 1) * 128, hh * 1024:(hh + 1) * 1024], writes=[res[t4]])
            for n in range(4):
                wv = wout_d.rearrange("(kc p) n -> p kc n", p=128)
                w = load_cg(lambda k0, nn, n=n: wv[:, k0:k0 + nn, n * 512:(n + 1) * 512])

                def rcons(t4, ps, n=n):
                    stt(res[t4][:, n * 512:(n + 1) * 512], res[t4][:, n * 512:(n + 1) * 512], ALPHA, ps[:, :], ALU.mult, ALU.add, [res[t4], ps], [res[t4]])
                tm_group(w, rcons, lhs=mrgT)
            for t4 in range(NTT):
                tile_i = blk_idx * NTT + t4
                r_ = res[t4][:]
                if debug:
                    dma(SP, dbg["res"][t0 + t4 * 128:t0 + (t4 + 1) * 128, :], r_, reads=[res[t4]])
                layer_norm(r_, res[t4], ln1, lst, lmv)
                dma(SP, X1_d[t0 + t4 * 128:t0 + (t4 + 1) * 128, :], r_, reads=[res[t4]], writes=[x1d_buf])
                if debug:
                    dma(SP, dbg["x1"][t0 + t4 * 128:t0 + (t4 + 1) * 128, :], r_, reads=[res[t4]])
                act(x1b[:], r_, AF.Copy, [res[t4]], [x1b])
                for half in range(2):
                    ps_t = psum()
                    for kc in range(8):
                        k = half * 8 + kc
                        tr(ps_t[:, kc * 64:(kc + 1) * 64].bitcast(BF16), x1b[:, k * 128:(k + 1) * 128], ident_bf, [x1b, cm_bf], [ps_t])
                    cp(x1T[:, half * 8:(half + 1) * 8, :], ps_t[:, :].bitcast(BF16).rearrange("p (k t) -> p k t", k=8), [ps_t], [x1T])
                ps_l = psum()
                for k in range(KC):
                    mm(ps_l[:, 0:36], x1T[:, k, :], wr[:, k, :], k == 0, k == KC - 1, [x1T, wr], [ps_l])
                routing(tile_i, ps_l, rt, rti, abf, x1b)
                if debug:
                    cp(rt[:, 160:162], gates[:, tile_i, :], [gates], [rt])
                    cp(rt[:, 162:164], slots[:, tile_i, :], [slots], [rt])
                    dma(SP, dbg["rt"][t0 + t4 * 128:t0 + (t4 + 1) * 128, :], rt[:, 160:164], reads=[rt])
            barrier(skip=(POOL,))

        def layer_norm(r_, rT, lnw, lst, lmv):
            for q in range(4):
                op(DVE, lambda e, q=q: e.bn_stats(out=lst[:, q, :], in_=r_[:, q * 512:(q + 1) * 512]), reads=[rT], writes=[lst], acc=True)
            op(DVE, lambda e: e.bn_aggr(out=lmv[:, 0:2], in_=lst[:].rearrange("p a b -> p (a b)")), reads=[lst], writes=[lmv])
            act(lmv[:, 2:3], lmv[:, 1:2], AF.Sqrt, [lmv], [lmv], bias=LN_EPS)
            op(DVE, lambda e: e.reciprocal(out=lmv[:, 3:4], in_=lmv[:, 2:3]), reads=[lmv], writes=[lmv])
            stt(lmv[:, 4:5], lmv[:, 0:1], -1.0, lmv[:, 3:4], ALU.mult, ALU.mult, [lmv], [lmv])
            act(r_, r_, AF.Identity, [rT, lmv], [rT], bias=lmv[:, 4:5], scale=lmv[:, 3:4])
            tt(r_, r_, lnw[:, 0, :], ALU.mult, [rT, lnw], [rT])
            tt(r_, r_, lnw[:, 1, :], ALU.add, [rT, lnw], [rT])

        def routing(ti, ps_l, rt, rti, abf, x1b):
            R, W_ = [ps_l, rt, brt], [rt]
            lg = rt[:, 0:36]
            tt(lg, ps_l[:, 0:36], brt[:], ALU.add, R, W_)
            op(DVE, lambda e: e.tensor_reduce(out=rt[:, 36:37], in_=rt[:, 0:4], axis=AX.X, op=ALU.max), reads=[rt], writes=[rt])
            ts(rt[:, 37:38], rt[:, 36:37], -1.0, None, ALU.mult, None, [rt], [rt])
            op(ACT, lambda e: e.activation(out=rt[:, 40:44], in_=rt[:, 0:4], func=AF.Exp, bias=rt[:, 37:38], accum_out=rt[:, 38:39]), reads=[rt], writes=[rt])
            op(DVE, lambda e: e.reciprocal(out=rt[:, 39:40], in_=rt[:, 38:39]), reads=[rt], writes=[rt])
            ts(rt[:, 44:48], rt[:, 0:4], rt[:, 36:37], None, ALU.is_equal, None, [rt], [rt])
            ts(rt[:, 44:48], rt[:, 44:48], 1e30, -1e30, ALU.mult, ALU.add, [rt], [rt])
            for g in range(4):
                ts(rt[:, 48 + 8 * g:56 + 8 * g], rt[:, 4 + 8 * g:12 + 8 * g], rt[:, 44 + g:45 + g], None, ALU.add, None, [rt], [rt])
            lem = rt[:, 48:80]
            op(DVE, lambda e: e.max(out=rt[:, 80:88], in_=lem), reads=[rt], writes=[rt])
            op(DVE, lambda e: e.max_index(out=rti[:, 0:8], in_max=rt[:, 80:88], in_values=lem), reads=[rt], writes=[rti])
            tt(rt[:, 88:89], rt[:, 80:81], rt[:, 81:82], ALU.subtract, [rt], [rt])
            act(rt[:, 89:90], rt[:, 88:89], AF.Sigmoid, [rt], [rt])
            tt(gates[:, ti, 0:1], rt[:, 39:40], rt[:, 89:90], ALU.mult, [rt], [gates])
            tt(gates[:, ti, 1:2], rt[:, 39:40], gates[:, ti, 0:1], ALU.subtract, [rt, gates], [gates])
            ts(rt[:, 90:122], lem, rt[:, 80:81], None, ALU.is_equal, None, [rt], [rt])
            oh1 = rt[:, 90:122]
            ts(rt[:, 0:32], lem, rt[:, 81:82], None, ALU.is_equal, None, [rt], [rt])
            oh2 = rt[:, 0:32]
            tt(abf[:], oh1, oh2, ALU.add, [rt], [abf])
            ps_c = psum()
            mm(ps_c[:, 0:32], lstr_bf, abf[:], True, False, [cm_bf, abf], [ps_c])
            mm(ps_c[:, 0:32], ones_bf, acum[:], False, True, [cm_bf, acum], [ps_c])
            tt(rt[:, 122:154], ps_c[:, 0:32], oh1, ALU.mult, [ps_c, rt], [rt])
            op(DVE, lambda e: e.tensor_reduce(out=rt[:, 154:155], in_=rt[:, 122:154], axis=AX.X, op=ALU.add), reads=[rt], writes=[rt])
            tt(rt[:, 122:154], ps_c[:, 0:32], oh2, ALU.mult, [ps_c, rt], [rt])
            op(DVE, lambda e: e.tensor_reduce(out=rt[:, 155:156], in_=rt[:, 122:154], axis=AX.X, op=ALU.add), reads=[rt], writes=[rt])
            tt(acum[:], acum[:], abf[:], ALU.add, [acum, abf], [acum])
            cp(rt[:, 156:158], rti[:, 0:2], [rti], [rt])
            stt(rt[:, 158:160], rt[:, 156:158], float(CAP), rt[:, 154:156], ALU.mult, ALU.add, [rt], [rt])
            cp(slots[:, ti, :], rt[:, 158:160], [rt], [slots])
            for k in range(2):
                op(POOL, lambda e, k=k: e.indirect_dma_start(out=XE_d[:, :], out_offset=bass.IndirectOffsetOnAxis(ap=slots[:, ti, k:k + 1], axis=0),
                                                             in_=x1b[:, :], in_offset=None),
                   reads=[x1b, slots], writes=[xe_buf], dma=True, acc=True)

        xe_buf = T(None, "XE")
        x1d_buf = T(None, "X1")
        ye_buf = T(None, "YE")

        for b in range(NPRE):
            block(b * TB, "prelast" if b == NPRE - 1 else "pre", -1)
        for h in range(NH):
            ts(Cst[h][:], Cst[h][:], flag[:, 0:1], None, ALU.mult, None, [Cst[h], flag], [Cst[h]])
        ts(mst[:], mst[:], flag[0:4, 0:1], None, ALU.mult, None, [mst, flag], [mst])
        ts(qkcar[:].rearrange("p a b -> p (a b)"), qkcar[:].rearrange("p a b -> p (a b)"), flag[:, 0:1], None, ALU.mult, None, [qkcar, flag], [qkcar])
        ts(ucar[:].rearrange("p a b -> p (a b)"), ucar[:].rearrange("p a b -> p (a b)"), flag[:, 0:1], None, ALU.mult, None, [ucar, flag], [ucar])
        for b in range(NBLK):
            block(TOK + b * TB, "main", b)

        barrier()
        ar.reset(g_mark)
        gu = [ar.alloc([128, 2, KC, 384], BF16, f"gu{i}") for i in range(2)]
        wdn = [ar.alloc([128, 6, D], BF16, f"wdn{i}") for i in range(2)]
        xg = [ar.alloc([128, D], BF16, f"xg{i}") for i in range(2)]
        xTe = [ar.alloc([128, KC, CAP], BF16, f"xTe{i}") for i in range(2)]
        hidT = [ar.alloc([128, 6, CAP], BF16, f"hidT{i}") for i in range(2)]
        sgt = [ar.alloc([128, CAP], F32, f"sgt{i}") for i in range(2)]
        ysb = [ar.alloc([128, D], F32, f"ysb{i}") for i in range(2)]
        ui = 0
        for e_ in range(NE):
            xTe_ = xTe[e_ % 2]
            hid_ = hidT[e_ % 2]
            for st_ in range(2):
                xg_ = xg[st_]
                r0 = e_ * CAP + st_ * 128
                dma(SP, xg_[:], XE_d[r0:r0 + 128, :], reads=[xe_buf], writes=[xg_])
                for half in range(2):
                    ps_t = psum()
                    for kc in range(8):
                        k = half * 8 + kc
                        tr(ps_t[:, kc * 64:(kc + 1) * 64].bitcast(BF16), xg_[:, k * 128:(k + 1) * 128], ident_bf, [xg_, cm_bf], [ps_t])
                    cp(xTe_[:, half * 8:(half + 1) * 8, st_ * 128:(st_ + 1) * 128], ps_t[:, :].bitcast(BF16).rearrange("p (k t) -> p k t", k=8),
                       [ps_t], [xTe_])
            for half in range(2):
                gu_ = gu[ui % 2]
                ui += 1
                for mi, wsrc in ((0, wg_d), (1, wu_d)):
                    wv = wsrc[e_].rearrange("(kc p) n -> p kc n", p=128)
                    for k0 in range(0, KC, 4):
                        dma(POOL, gu_[:, mi, k0:k0 + 4, :], wv[:, k0:k0 + 4, half * 384:(half + 1) * 384], writes=[gu_])
                for fc in range(3):
                    psg, psu = psum(), psum()
                    for k in range(KC):
                        mm(psg[:, 0:CAP], gu_[:, 0, k, fc * 128:(fc + 1) * 128], xTe_[:, k, :], k == 0, k == KC - 1, [gu_, xTe_], [psg])
                    for k in range(KC):
                        mm(psu[:, 0:CAP], gu_[:, 1, k, fc * 128:(fc + 1) * 128], xTe_[:, k, :], k == 0, k == KC - 1, [gu_, xTe_], [psu])
                    sg_ = sgt[fc % 2]
                    act(sg_[:], psg[:, 0:CAP], AF.Silu, [psg], [sg_])
                    tt(hid_[:, half * 3 + fc, :], sg_[:], psu[:, 0:CAP], ALU.mult, [sg_, psu], [hid_])
            wd_ = wdn[e_ % 2]
            wdv = wd_d[e_].rearrange("(f p) n -> p f n", p=128)
            for f0 in range(0, 6, 2):
                for hh in range(2):
                    dma(POOL, wd_[:, f0:f0 + 2, hh * 1024:(hh + 1) * 1024], wdv[:, f0:f0 + 2, hh * 1024:(hh + 1) * 1024], writes=[wd_])
            for st_ in range(2):
                y_ = ysb[st_]
                for n in range(4):
                    ps = psum()
                    for f in range(6):
                        mm(ps[:, :], hid_[:, f, st_ * 128:(st_ + 1) * 128], wd_[:, f, n * 512:(n + 1) * 512], f == 0, f == 5, [hid_, wd_], [ps])
                    if n % 2 == 0:
                        act(y_[:, n * 512:(n + 1) * 512], ps[:, :], AF.Copy, [ps], [y_], acc=True)
                    else:
                        op(DVE, lambda e, y_=y_, n=n, ps=ps: e.tensor_copy(out=y_[:, n * 512:(n + 1) * 512], in_=ps[:, :]), reads=[ps], writes=[y_], acc=True)
                r0 = e_ * CAP + st_ * 128
                for hh in range(2):
                    dma(SP, YE_d[r0:r0 + 128, hh * 1024:(hh + 1) * 1024], y_[:, hh * 1024:(hh + 1) * 1024], reads=[y_], writes=[ye_buf])

        barrier()
        ar.reset(g_mark)
        wpg = ar.alloc([128, KC, D], BF16, "wpg")
        wpp = ar.alloc([128, 2, D], BF16, "wpp")
        bpg = ar.alloc([128, D], F32, "bpg")
        ln2 = ar.alloc([128, 2, D], F32, "ln2")
        pTt = ar.alloc([128, 2, TOK], BF16, "pTt")
        wpgv = wpg_d.rearrange("(kc p) n -> p kc n", p=128)
        for k in range(KC):
            for hh in range(2):
                dma(POOL, wpg[:, k, hh * 1024:(hh + 1) * 1024], wpgv[:, k, hh * 1024:(hh + 1) * 1024], writes=[wpg])
        wppv = wpp_d.rearrange("(kc p) n -> p kc n", p=128)
        pTv = pT_d.rearrange("(kc p) n -> p kc n", p=128)
        for kc in range(2):
            for hh in range(2):
                dma(POOL, wpp[:, kc, hh * 1024:(hh + 1) * 1024], wppv[:, kc, hh * 1024:(hh + 1) * 1024], writes=[wpp])
                dma(POOL, pTt[:, kc, hh * 1024:(hh + 1) * 1024], pTv[:, kc, hh * 1024:(hh + 1) * 1024], writes=[pTt])
        dma(SP, bpg[:], bpg_d, writes=[bpg])
        for i in range(2):
            dma(SP, ln2[:, i, :], ln_d[:, 2 + i, :], writes=[ln2])
        Y1 = [ar.alloc([128, D], F32, f"Y1_{i}") for i in range(2)]
        Y2 = [ar.alloc([128, D], F32, f"Y2_{i}") for i in range(2)]
        xr = [ar.alloc([128, D], F32, f"xr{i}") for i in range(2)]
        x2b = [ar.alloc([128, D], BF16, f"x2b{i}") for i in range(2)]
        x2T = [ar.alloc([128, KC, 128], BF16, f"x2T{i}") for i in range(2)]
        ot = [ar.alloc([128, D], F32, f"ot{i}") for i in range(2)]
        tg = [ar.alloc([128, 512], F32, f"tg{i}") for i in range(2)]
        lst = ar.alloc([128, 4, 6], F32, "lst2")
        lmv = ar.alloc([128, 8], F32, "lmv2")
        finals = []

        def c_a1(ti):
            y1, y2, x_ = Y1[ti % 2], Y2[ti % 2], xr[ti % 2]
            r0 = ti * 128
            for k, yk in ((0, y1), (1, y2)):
                op(POOL, lambda e, k=k, yk=yk, ti=ti: e.indirect_dma_start(out=yk[:, :], out_offset=None, in_=YE_d[:, :],
                                                                          in_offset=bass.IndirectOffsetOnAxis(ap=slots[:, ti, k:k + 1], axis=0)),
                   reads=[ye_buf, slots], writes=[yk], dma=True, acc=False)
            for hh in range(2):
                dma(SP, x_[:, hh * 1024:(hh + 1) * 1024], X1_d[r0:r0 + 128, hh * 1024:(hh + 1) * 1024], reads=[x1d_buf], writes=[x_])
            act(x_[:], x_[:], AF.Copy, [x_], [x_], scale=ALPHA)
            if debug:
                o_ = ot[ti % 2]
                ts(o_[:], y1[:], gates[:, ti, 0:1], None, ALU.mult, None, [y1, gates], [o_])
                stt(o_[:], y2[:], gates[:, ti, 1:2], o_[:], ALU.mult, ALU.add, [y2, gates, o_], [o_])
                dma(SP, dbg["moe"][r0:r0 + 128, :], o_[:], reads=[o_])
            stt(x_[:], y1[:], gates[:, ti, 0:1], x_[:], ALU.mult, ALU.add, [y1, gates, x_], [x_])
            stt(x_[:], y2[:], gates[:, ti, 1:2], x_[:], ALU.mult, ALU.add, [y2, gates, x_], [x_])
            layer_norm(x_[:], x_, ln2, lst, lmv)
            act(x2b[ti % 2][:], x_[:], AF.Copy, [x_], [x2b[ti % 2]])

        def c_a2(ti):
            xb_, xT_ = x2b[ti % 2], x2T[ti % 2]
            for half in range(2):
                ps_t = psum()
                for kc in range(8):
                    k = half * 8 + kc
                    tr(ps_t[:, kc * 64:(kc + 1) * 64].bitcast(BF16), xb_[:, k * 128:(k + 1) * 128], ident_bf, [xb_, cm_bf], [ps_t])
                cp(xT_[:, half * 8:(half + 1) * 8, :], ps_t[:, :].bitcast(BF16).rearrange("p (k t) -> p k t", k=8), [ps_t], [xT_], eng=ACT_OR_DVE[half])

        def c_b(ti):
            x_, o_, xT_ = xr[ti % 2], ot[ti % 2], x2T[ti % 2]
            r0 = ti * 128
            for n in range(4):
                ns = slice(n * 512, (n + 1) * 512)
                psg, psp = psum(), psum()
                for k in range(KC):
                    mm(psg[:, :], xT_[:, k, :], wpg[:, k, ns], k == 0, k == KC - 1, [xT_, wpg], [psg])
                for kc in range(2):
                    mm(psp[:, :], pTt[:, kc, r0:r0 + 128], wpp[:, kc, ns], kc == 0, kc == 1, [pTt, wpp], [psp])
                t_ = tg[n % 2]
                tt(t_[:], psg[:, :], bpg[:, ns], ALU.add, [psg, bpg], [t_])
                act(t_[:], t_[:], AF.Sigmoid, [t_], [t_])
                tt(t_[:], t_[:], psp[:, :], ALU.mult, [t_, psp], [t_])
                op(DVE, lambda e, o_=o_, t_=t_, x_=x_, ns=ns: e.tensor_tensor(out=o_[:, ns], in0=t_[:], in1=x_[:, ns], op=ALU.add), reads=[t_, x_], writes=[o_], acc=True)
            for hh in range(2):
                finals.append(dma(SP, out_d[r0:r0 + 128, hh * 1024:(hh + 1) * 1024], o_[:, hh * 1024:(hh + 1) * 1024], reads=[o_]))

        ACT_OR_DVE = (DVE, DVE)
        c_a1(0)
        c_a2(0)
        for ti in range(16):
            if ti + 1 < 16:
                c_a1(ti + 1)
            c_b(ti)
            if ti + 1 < 16:
                c_a2(ti + 1)
        s.emit(final_waits=finals)
    return nc


def _prep_shared(inp):
    f = np.float32
    w_in = np.ascontiguousarray(inp["w_in"][0], dtype=f)
    b_in = np.asarray(inp["b_in"][0], dtype=f)
    sh = {}
    sh["w_in"] = w_in
    w_if = np.zeros((D, 64), f)
    w_if[:, 0:4] = w_in[:, 3072:3076]
    w_if[:, 32:36] = w_in[:, 3076:3080]
    sh["w_if"] = w_if
    bfm = np.zeros((128, 56), f)
    bfm[:, 0:8] = b_in[0:1024].reshape(8, 128).T
    bfm[:, 8:16] = b_in[OFF_U:OFF_U + 1024].reshape(8, 128).T
    bfm[:, 16:32] = b_in[OFF_GM:OFF_GM + 2048].reshape(16, 128).T
    bfm[:, 32:48] = b_in[OFF_GP:OFF_GP + 2048].reshape(16, 128).T
    bfm[0:4, 48] = b_in[3072:3076]
    bfm[0:4, 49] = b_in[3076:3080]
    sh["b_fm"] = bfm
    sh["b_tm"] = np.ascontiguousarray(np.broadcast_to(b_in[1024:3072][None, :], (128, 2048)), dtype=f)
    cw = np.zeros((128, 8, 5), f)
    conv_w = np.asarray(inp["conv_w"][0], dtype=f)
    conv_b = np.asarray(inp["conv_b"][0], dtype=f)
    cw[:, :, 0:4] = conv_w.reshape(4, 8, 128).transpose(2, 1, 0)
    cw[:, :, 4] = conv_b.reshape(8, 128).T
    sh["convw"] = cw
    sh["mhg"] = np.ascontiguousarray(np.broadcast_to(np.asarray(inp["mh_g"][0], f)[None, :], (128, 1024)))
    sh["pool_sc"] = np.ascontiguousarray(np.asarray(inp["pool_scale"][0], f).reshape(8, 128).T)
    sh["w_pool"] = np.ascontiguousarray(inp["w_pool"][0], dtype=f)
    sh["w_m_br"] = np.ascontiguousarray(inp["w_m_br"][0], dtype=f)
    sh["w_p_br"] = np.ascontiguousarray(inp["w_p_br"][0], dtype=f)
    sh["w_out"] = np.ascontiguousarray(inp["w_out"][0], dtype=f)
    ln = np.stack([inp["ln1_g"][0], inp["ln1_b"][0], inp["ln2_g"][0], inp["ln2_b"][0]], 0).astype(f)
    sh["ln"] = np.ascontiguousarray(np.broadcast_to(ln[None], (128, 4, D)))
    sh["w_r"] = np.ascontiguousarray(np.concatenate([inp["w_rg"][0], inp["w_re"][0]], axis=1), dtype=f)
    b_r = np.concatenate([inp["b_rg"][0], inp["b_re"][0]]).astype(f)
    sh["b_r"] = np.ascontiguousarray(np.broadcast_to(b_r[None, :], (128, 36)))
    sh["w_gate"] = np.ascontiguousarray(inp["w_gate"][0], dtype=f)
    sh["w_up"] = np.ascontiguousarray(inp["w_up"][0], dtype=f)
    sh["w_down"] = np.ascontiguousarray(inp["w_down"][0], dtype=f)
    sh["w_ple_gate"] = np.ascontiguousarray(inp["w_ple_gate"][0], dtype=f)
    sh["b_pg"] = np.ascontiguousarray(np.broadcast_to(np.asarray(inp["b_ple_gate"][0], f)[None, :], (128, D)))
    sh["w_ple_proj"] = np.ascontiguousarray(inp["w_ple_proj"][0], dtype=f)
    cm = np.zeros((128, 4, 128), f)
    cm[:, 0, :] = np.eye(128, dtype=f)
    cm[:, 1, :] = np.triu(np.ones((128, 128), f))
    cm[:, 2, :] = np.triu(np.ones((128, 128), f), k=1)
    cm[:, 3, :] = 1.0
    sh["cmat"] = cm
    return sh


def _prep_core(inp, c):
    f = np.float32
    b, half = c // 2, c % 2
    x = np.asarray(inp["x"], dtype=f)
    p = np.asarray(inp["p"], dtype=f)
    xT = np.zeros((D, 2 * TOK), f)
    if half == 1:
        xT[:, 0:TOK] = x[b, 0:TOK, :].T
    xT[:, TOK:] = x[b, half * TOK:(half + 1) * TOK, :].T
    m = {}
    m["xT"] = xT
    m["xtok"] = np.ascontiguousarray(x[b, half * TOK:(half + 1) * TOK, :])
    m["pT"] = np.ascontiguousarray(p[0, b, half * TOK:(half + 1) * TOK, :].T)
    m["flag"] = np.full((128, 1), float(half), f)
    return m


_CACHE = {}


def kernel(**inputs):
    debug = bool(inputs.pop("_debug", False))
    cores = inputs.pop("_cores", list(range(8)))
    import time as _time
    _t0 = _time.time()
    key = ("nc", debug)
    if key not in _CACHE:
        _CACHE[key] = build_program(debug)
    nc = _CACHE[key]
    _t1 = _time.time()
    sh = _prep_shared(inputs)
    in_maps = []
    for c in cores:
        m = dict(sh)
        m.update(_prep_core(inputs, c))
        in_maps.append(m)
    _t2 = _time.time()
    res = run_bass_kernel_spmd(nc, in_maps, core_ids=list(range(len(cores))))
    print(f"[kernel] build {_t1 - _t0:.1f}s prep {_t2 - _t1:.1f}s run {_time.time() - _t2:.1f}s", flush=True)
    if debug:
        return res.results
    out = np.zeros((4, 4096, D), np.float32)
    for i, c in enumerate(cores):
        b, half = c // 2, c % 2
        out[b, half * TOK:(half + 1) * TOK, :] = res.results[i]["out"]
    return out
```

```python
import contextlib
import numpy as np
import concourse.bass as bass
import concourse.mybir as mybir
from concourse.bass_utils import run_bass_kernel_spmd

F32 = mybir.dt.float32
BF16 = mybir.dt.bfloat16
I32 = mybir.dt.int32
U32 = mybir.dt.uint32
AF = mybir.ActivationFunctionType
ALU = mybir.AluOpType
AX = mybir.AxisListType

PE, ACT, DVE, POOL, SP = "tensor", "scalar", "vector", "gpsimd", "sync"
COMPUTE = (PE, ACT, DVE, POOL)
ALL_ENG = (PE, ACT, DVE, POOL, SP)

D = 2048
KC = 16
TB = 512
NTT = 4
NBLK = 4
NPRE = 4
TOK = 2048
NH = 4
DV = 256
DVA = 257
CAP = 256
NE = 32
DE = 768
ALPHA = 2.0 ** 0.25
LN_EPS = 1e-5
LNS = float(np.log(128.0 ** -0.5))
WINS = (2, 4, 8, 16)
OFF_Q, OFF_K, OFF_V, OFF_O, OFF_U, OFF_GM, OFF_GP = 0, 512, 1024, 2048, 3080, 4104, 6152


class Buf:
    __slots__ = ("name", "w", "r", "pr")

    def __init__(self, name=""):
        self.name = name
        self.w = []
        self.r = []
        self.pr = []


class Op:
    __slots__ = ("eng", "fn", "deps", "is_dma", "need_sig", "sem", "val", "dma_prev")

    def __init__(self, eng, fn, is_dma):
        self.eng = eng
        self.fn = fn
        self.deps = []
        self.is_dma = is_dma
        self.need_sig = False
        self.sem = None
        self.val = None
        self.dma_prev = None


class Sched:
    def __init__(self, nc, n_dma_sems=48):
        self.nc = nc
        self.ops = {e: [] for e in ALL_ENG}
        self.n_dma_sems = n_dma_sems
        self.all_ops = []
        self.last = {e: None for e in ALL_ENG}
        self.dmas_since_barrier = []
        self.pending_barrier = {e: None for e in ALL_ENG}

    def op(self, eng, fn, reads=(), writes=(), dma=False, acc=False, extra_deps=(), ring=False):
        o = Op(eng, fn, dma)
        deps = []
        for b in reads:
            deps.extend(b.w)
        for b in writes:
            deps.extend(b.r)
            if acc and not b.r:
                deps.extend(b.pr)
            if not acc:
                deps.extend(b.w)
        deps.extend(extra_deps)
        if self.pending_barrier[eng] is not None:
            deps.append(self.pending_barrier[eng])
            self.pending_barrier[eng] = None
        seen = set()
        for d in deps:
            if d is o or id(d) in seen:
                continue
            seen.add(id(d))
            o.deps.append(d)
        for b in reads:
            b.r.append(o)
        for b in writes:
            if b.r or not acc:
                if b.r:
                    b.pr = b.r
                b.w = [o]
                b.r = []
            else:
                b.w.append(o)
        self.ops[eng].append(o)
        self.all_ops.append(o)
        self.last[eng] = o
        if dma and not ring:
            self.dmas_since_barrier.append(o)
        return o

    def barrier(self, fn, skip=()):
        deps = [self.last[e] for e in COMPUTE if self.last[e] is not None and e not in skip]
        deps += self.dmas_since_barrier
        self.dmas_since_barrier = []
        o = self.op(DVE, fn, extra_deps=deps)
        for e in ALL_ENG:
            if e != DVE and e not in skip:
                self.pending_barrier[e] = o
        return o

    def emit(self, final_waits=()):
        nc = self.nc
        for o in self.all_ops:
            kept = []
            for d in o.deps:
                if (not d.is_dma) and (not o.is_dma) and d.eng == o.eng and o.eng == PE:
                    continue
                kept.append(d)
            o.deps = kept
            for d in kept:
                d.need_sig = True
        for o in final_waits:
            o.need_sig = True
        with contextlib.ExitStack() as st:
            eng_sem = {e: st.enter_context(nc.semaphore(f"s_{e}")) for e in COMPUTE}
            cnt = {e: 0 for e in COMPUTE}
            dma_sems = [st.enter_context(nc.semaphore(f"s_dma_{i}")) for i in range(self.n_dma_sems)]
            dma_cnt = [0] * self.n_dma_sems
            dma_last = [None] * self.n_dma_sems
            di = 0
            for o in self.all_ops:
                if o.is_dma:
                    k = di % self.n_dma_sems
                    di += 1
                    o.dma_prev = dma_last[k]
                    dma_cnt[k] += 16
                    o.sem, o.val = dma_sems[k], dma_cnt[k]
                    dma_last[k] = o
                    o.need_sig = True
                elif o.need_sig:
                    cnt[o.eng] += 1
                    o.sem, o.val = eng_sem[o.eng], cnt[o.eng]
            blk = st.enter_context(nc.Block())

            def make(eng_name):
                ops = self.ops[eng_name]

                def body(eng):
                    waited = {}

                    def wait(d):
                        key = id(d.sem)
                        if waited.get(key, 0) >= d.val:
                            return
                        eng.wait_ge(d.sem, d.val)
                        waited[key] = d.val
                    for o in ops:
                        for d in o.deps:
                            wait(d)
                        if o.is_dma and o.dma_prev is not None:
                            wait(o.dma_prev)
                        ins = o.fn(eng)
                        if o.need_sig:
                            ins.then_inc(o.sem, 16 if o.is_dma else 1)
                    if eng_name == SP:
                        for o in final_waits:
                            wait(o)
                return body
            blk.tensor(make(PE))
            blk.scalar(make(ACT))
            blk.vector(make(DVE))
            blk.gpsimd(make(POOL))
            blk.sync(make(SP))


class T:
    __slots__ = ("ap", "buf")

    def __init__(self, ap, name=""):
        self.ap = ap
        self.buf = Buf(name)

    def __getitem__(self, k):
        return self.ap[k]


class Arena:
    def __init__(self, ap_f32, nbytes):
        self.base = ap_f32
        self.nbytes = nbytes
        self.off = 0
        self.marks = []

    def alloc(self, shape, dt, name=""):
        esz = 2 if dt == BF16 else 4
        n = 1
        for x in shape[1:]:
            n *= x
        nb = (n * esz + 31) // 32 * 32
        assert self.off + nb <= self.nbytes, f"arena overflow allocating {name}: {self.off}+{nb}>{self.nbytes}"
        v = self.base[0:shape[0], self.off // 4:(self.off + nb) // 4]
        self.off += nb
        if dt != F32:
            v = v.bitcast(dt)
        v = v[:, 0:n]
        if len(shape) == 3:
            v = v.rearrange("p (a b) -> p a b", a=shape[1])
        elif len(shape) == 4:
            v = v.rearrange("p (a b c) -> p a b c", a=shape[1], b=shape[2])
        return T(v, name)

    def mark(self):
        return self.off

    def reset(self, m):
        self.off = m


def build_program(debug=False):
    nc = bass.Bass("TRN2", target_bir_lowering=False)

    def din(name, shape, dt=F32):
        return nc.dram_tensor(name, list(shape), dt, kind="ExternalInput").ap()

    xT_d = din("xT", [D, 2 * TOK])
    xtok_d = din("xtok", [TOK, D])
    pT_d = din("pT", [256, TOK])
    flag_d = din("flag", [128, 1])
    cmat_d = din("cmat", [128, 4, 128])
    w_in_d = din("w_in_cg", [16, 128, KC, 512])
    w_if_d = din("w_if", [128, KC, 64])
    bfm_d = din("b_fm", [128, 56])
    btm_d = din("b_tm", [128, 2048])
    convw_d = din("convw", [128, 8, 5])
    mhg_d = din("mhg", [128, 1024])
    poolsc_d = din("pool_sc", [128, 8])
    wpool_d = din("w_pool", [4, 256, 256])
    wbr_d = din("w_br", [4, 128, KC, 512])
    wout_d = din("w_out_cg", [4, 128, KC, 512])
    ln_d = din("ln", [128, 4, D])
    wr_d = din("w_r", [D, 36])
    br_d = din("b_r", [128, 36])
    wg_d = din("w_gate", [NE, D, DE])
    wu_d = din("w_up", [NE, D, DE])
    wd_d = din("w_down", [NE, DE, D])
    wpg_d = din("w_ple_gate", [D, D])
    bpg_d = din("b_pg", [128, D])
    wpp_d = din("w_ple_proj", [256, D])
    out_d = nc.dram_tensor("out", [TOK, D], F32, kind="ExternalOutput").ap()
    XE_d = nc.dram_tensor("XE", [NE * CAP, D], BF16, kind="Internal").ap()
    YE_d = nc.dram_tensor("YE", [NE * CAP, D], F32, kind="Internal").ap()
    X1_d = nc.dram_tensor("X1", [TOK, D], F32, kind="Internal").ap()
    dbg = {}
    if debug:
        dbg["x1"] = nc.dram_tensor("dbg_x1", [TOK, D], F32, kind="ExternalOutput").ap()
        dbg["h"] = nc.dram_tensor("dbg_h", [TOK, 1024], F32, kind="ExternalOutput").ap()
        dbg["mrg"] = nc.dram_tensor("dbg_mrg", [D, TOK], F32, kind="ExternalOutput").ap()
        dbg["rt"] = nc.dram_tensor("dbg_rt", [TOK, 4], F32, kind="ExternalOutput").ap()
        dbg["moe"] = nc.dram_tensor("dbg_moe", [TOK, D], F32, kind="ExternalOutput").ap()
        dbg["res"] = nc.dram_tensor("dbg_res", [TOK, D], F32, kind="ExternalOutput").ap()

    ARENA_BYTES = 204800
    with contextlib.ExitStack() as st:
        arena_t = st.enter_context(nc.sbuf_tensor("arena", [128, ARENA_BYTES // 4], F32))
        ar = Arena(arena_t, ARENA_BYTES)
        psum_t = [st.enter_context(nc.psum_tensor(f"ps{i}", [128, 512], F32)) for i in range(8)]
        banks = [T(psum_t[i][:, :], f"bank{i}") for i in range(8)]
        s = Sched(nc)
        state = {"pi": 0, "pool": list(range(8))}

        def psum():
            pool = state["pool"]
            b = banks[pool[state["pi"] % len(pool)]]
            state["pi"] += 1
            return b

        def op(eng, fn, reads=(), writes=(), **kw):
            return s.op(eng, fn, reads=[t.buf for t in reads], writes=[t.buf for t in writes], **kw)

        def dma(eng, out_ap, in_ap, reads=(), writes=(), acc=True, ring=False):
            return op(eng, lambda e: e.dma_start(out=out_ap, in_=in_ap), reads=reads, writes=writes, dma=True, acc=acc, ring=ring)

        def act(out_ap, in_ap, func, reads, writes, bias=None, scale=None, **kw):
            def f(e):
                kws = {}
                if bias is not None:
                    kws["bias"] = bias
                if scale is not None:
                    kws["scale"] = scale
                return e.activation(out=out_ap, in_=in_ap, func=func, **kws)
            return op(ACT, f, reads=reads, writes=writes, **kw)

        def tt(out_ap, in0, in1, alu, reads, writes, eng=DVE):
            return op(eng, lambda e: e.tensor_tensor(out=out_ap, in0=in0, in1=in1, op=alu), reads=reads, writes=writes)

        def ts(out_ap, in0, s1, s2, op0, op1, reads, writes, eng=DVE):
            if s2 is None:
                return op(eng, lambda e: e.tensor_scalar(out=out_ap, in0=in0, scalar1=s1, scalar2=None, op0=op0), reads=reads, writes=writes)
            return op(eng, lambda e: e.tensor_scalar(out=out_ap, in0=in0, scalar1=s1, scalar2=s2, op0=op0, op1=op1), reads=reads, writes=writes)

        def stt(out_ap, in0, sc, in1, op0, op1, reads, writes):
            return op(DVE, lambda e: e.scalar_tensor_tensor(out=out_ap, in0=in0, scalar=sc, in1=in1, op0=op0, op1=op1), reads=reads, writes=writes)

        def cp(out_ap, in_ap, reads, writes, eng=DVE):
            return op(eng, lambda e: e.tensor_copy(out=out_ap, in_=in_ap), reads=reads, writes=writes)

        def mm(out_ap, lhsT, rhs, start, stop, reads, writes):
            return op(PE, lambda e: e.matmul(out_ap, lhsT=lhsT, rhs=rhs, start=start, stop=stop), reads=reads, writes=writes, acc=True)

        def tr(out_ap, in_ap, ident, reads, writes):
            return op(PE, lambda e: e.transpose(out=out_ap, in_=in_ap, identity=ident), reads=reads, writes=writes, acc=True)

        def memset(ap, val, writes, eng=DVE):
            return op(eng, lambda e: e.memset(ap, val), writes=writes)

        cm_bf = ar.alloc([128, 4, 128], BF16, "cm_bf")
        cm_f = ar.alloc([128, 4, 128], F32, "cm_f")
        flag = ar.alloc([128, 1], F32, "flag")
        slots = ar.alloc([128, 16, 2], I32, "slots")
        gates = ar.alloc([128, 16, 2], F32, "gates")
        acum = ar.alloc([128, 32], BF16, "acum")
        dummy = ar.alloc([128, 8], F32, "dummy")
        dma(SP, cm_f[:], cmat_d, writes=[cm_f])
        dma(POOL, cm_bf[:], cmat_d, writes=[cm_bf])
        dma(SP, flag[:], flag_d, writes=[flag])
        memset(acum[:], 0.0, [acum])
        ident_bf = cm_bf[:, 0, :]
        mask_bf = cm_bf[:, 1, :]
        lstr_bf = cm_bf[:, 2, :]
        ones_bf = cm_bf[:, 3, :]
        ident4 = cm_f[0:4, 0, 0:4]
        ones4 = cm_f[0:4, 3, :]
        g_mark = ar.mark()

        def barrier(skip=()):
            s.barrier(lambda e: e.memset(dummy[:, 0:1], 0.0), skip=skip)

        bfm = ar.alloc([128, 56], F32, "bfm")
        btm = ar.alloc([128, 2048], F32, "btm")
        convw = ar.alloc([128, 8, 5], F32, "convw")
        mhg = ar.alloc([128, 1024], F32, "mhg")
        poolsc = ar.alloc([128, 8], F32, "poolsc")
        wpool = ar.alloc([128, 4, 2, 256], BF16, "wpool")
        wr = ar.alloc([128, KC, 36], BF16, "wr")
        brt = ar.alloc([128, 36], F32, "brt")
        Cst = [ar.alloc([128, DVA], F32, f"Cst{h}") for h in range(NH)]
        Cs = [ar.alloc([128, DVA], F32, f"Cs{h}") for h in range(NH)]
        Csb = [ar.alloc([128, DVA], BF16, f"Csb{h}") for h in range(NH)]
        mst = ar.alloc([4, 1], F32, "mst")
        qkcar = ar.alloc([128, 8, 3], F32, "qkcar")
        ucar = ar.alloc([128, 8, 15], F32, "ucar")
        invc = ar.alloc([128, 4, 16], F32, "invc")
        negbf = ar.alloc([4, 1], F32, "negbf")
        ones_row = ar.alloc([4, TB], F32, "ones_row")
        dma(SP, bfm[:], bfm_d, writes=[bfm])
        dma(SP, btm[:], btm_d, writes=[btm])
        dma(SP, convw[:], convw_d, writes=[convw])
        dma(SP, mhg[:], mhg_d, writes=[mhg])
        dma(SP, poolsc[:], poolsc_d, writes=[poolsc])
        for g in range(4):
            dma(POOL, wpool[:, g, :, :], wpool_d[g].rearrange("(kc p) n -> p kc n", p=128), writes=[wpool])
        dma(POOL, wr[:], wr_d.rearrange("(kc p) n -> p kc n", p=128), writes=[wr])
        dma(SP, brt[:], br_d, writes=[brt])
        for h in range(NH):
            memset(Cst[h][:], 0.0, [Cst[h]])
        memset(mst[:], 0.0, [mst])
        memset(qkcar[:], 0.0, [qkcar])
        memset(ucar[:], 0.0, [ucar])
        memset(ones_row[:], 1.0, [ones_row])
        ts(negbf[:], bfm[0:4, 49:50], -1.0, None, ALU.mult, None, [bfm], [negbf])
        f2k = ar.alloc([128, 1], F32, "f2k")
        ts(f2k[:], flag[:], 2048.0, None, ALU.mult, None, [flag], [f2k])
        iot = ar.alloc([128, 16], F32, "iot")
        op(POOL, lambda e: e.iota(iot[:], pattern=[[1, 16]], base=1, channel_multiplier=0, allow_small_or_imprecise_dtypes=True), writes=[iot])
        for g in range(4):
            ts(invc[:, g, :], iot[:], f2k[:, 0:1], float(WINS[g]), ALU.add, ALU.min, [iot, f2k], [invc])
        op(DVE, lambda e: e.reciprocal(out=invc[:], in_=invc[:]), reads=[invc], writes=[invc])

        NW = 3
        wbuf = [ar.alloc([128, KC, 512], BF16, f"wbuf{i}") for i in range(NW)]
        wstate = {"i": 0}
        xT = ar.alloc([128, KC, TB], BF16, "xT")
        hT = ar.alloc([128, 8, TB], BF16, "hT")
        y2T = ar.alloc([128, 8, TB], BF16, "y2T")
        mrgT = ar.alloc([128, KC, TB], BF16, "mrgT")
        a_mark = ar.mark()

        def next_wbuf():
            w = wbuf[wstate["i"] % NW]
            wstate["i"] += 1
            return w

        def load_cg(src_ap_fn, nk=KC, ncols=512):
            w = next_wbuf()
            for k0 in range(0, nk, 4):
                dma(POOL, w[:, k0:k0 + 4, 0:ncols], src_ap_fn(k0, 4), writes=[w], ring=True)
            return w

        CG_OF = {OFF_Q: 0, OFF_K: 1, OFF_V: 2, OFF_V + 512: 3, OFF_O: 4, OFF_O + 512: 5, OFF_U: 6, OFF_U + 512: 7}
        for i_ in range(4):
            CG_OF[OFF_GM + i_ * 512] = 8 + i_
            CG_OF[OFF_GP + i_ * 512] = 12 + i_

        def win_cols(c0):
            v = w_in_d[CG_OF[c0]]
            return lambda k0, n: v[:, k0:k0 + n, :]

        def load_xT(tok0):
            v = xT_d.rearrange("(kc p) n -> p kc n", p=128)
            for k0 in range(0, KC, 4):
                dma(POOL, xT[:, k0:k0 + 4, :], v[:, k0:k0 + 4, tok0:tok0 + TB], writes=[xT], ring=True)

        def fm_group(w, ncc, consume):
            for cc in range(ncc):
                ps = psum()
                for k in range(KC):
                    mm(ps[:, :], w[:, k, cc * 128:(cc + 1) * 128], xT[:, k, :], k == 0, k == KC - 1, [w, xT], [ps])
                consume(cc, ps)

        def tm_group(w, consume, lhs=None):
            lhs = xT if lhs is None else lhs
            for t4 in range(NTT):
                ps = psum()
                for k in range(KC):
                    mm(ps[:, :], lhs[:, k, t4 * 128:(t4 + 1) * 128], w[:, k, :], k == 0, k == KC - 1, [w, lhs], [ps])
                consume(t4, ps)

        def block(tok0, kind, blk_idx):
            main = kind == "main"
            ar.reset(a_mark)
            gi = ar.alloc([4, TB], F32, "gi")
            bneg = ar.alloc([4, TB], F32, "bneg")
            aa = ar.alloc([4, TB], F32, "aa")
            arg1 = ar.alloc([4, TB], F32, "arg1")
            arg2 = ar.alloc([4, TB], F32, "arg2")
            small = ar.alloc([4, 32], F32, "small")
            EX = ar.alloc([128, NTT, 12], F32, "EX")
            pre = [ar.alloc([128, TB + 3], F32, f"pre{i}") for i in range(2)]
            cacc = [ar.alloc([128, TB], F32, f"cacc{i}") for i in range(2)]
            qkT = ar.alloc([128, 8, TB], BF16, "qkT")
            kp = [ar.alloc([128, 128], BF16, f"kp{i}") for i in range(4)]
            vaug = ar.alloc([128, NTT, NH, DVA], BF16, "vaug")
            load_xT(tok0)
            memset(vaug[:, :, :, 256:257], 1.0, [vaug])
            if main:
                gso = ar.alloc([128, NTT, 1024], BF16, "gso")
                otmp = [ar.alloc([128, 512], F32, f"otmp{i}") for i in range(2)]
                S_sb = [ar.alloc([128, 128], BF16, f"S{i}") for i in range(4)]
                htok = [ar.alloc([128, 1024], BF16, f"htok{i}") for i in range(2)]
                hnrm = [ar.alloc([128, 256], F32, f"hnrm{i}") for i in range(2)]
                stat = ar.alloc([128, NH, 6], F32, "stat")
                mv = ar.alloc([128, NH, 2], F32, "mv")
                sm = ar.alloc([128, 8, 4], F32, "sm")
            if kind != "pre":
                ubuf = [ar.alloc([128, TB + 15], F32, f"ubuf{i}") for i in range(2)]
                uw = [ar.alloc([128, TB + 15], F32, f"uw{i}") for i in range(2)]
                yT = [ar.alloc([128, 2, TB], BF16, f"yT{i}") for i in range(2)]

            w = load_cg(lambda k0, n: w_if_d[:, k0:k0 + n, :], ncols=64)
            psI, psF = psum(), psum()
            for k in range(KC):
                mm(psI[0:4, :], w[:, k, 0:4], xT[:, k, :], k == 0, k == KC - 1, [w, xT], [psI])
            for k in range(KC):
                mm(psF[0:4, :], w[:, k, 32:36], xT[:, k, :], k == 0, k == KC - 1, [w, xT], [psF])
            act(arg1[:], psF[0:4, :], AF.Exp, [psF, negbf], [arg1], bias=negbf[:, 0:1], scale=-1.0)
            act(arg2[:], arg1[:], AF.Ln, [arg1], [arg2], bias=1.0)
            act(gi[:], psI[0:4, :], AF.Identity, [psI, bfm], [gi], bias=bfm[0:4, 48:49])
            for c in range(NTT):
                cs_ = slice(c * 128, (c + 1) * 128)
                op(DVE, lambda e, cs_=cs_: e.tensor_tensor_scan(out=bneg[:, cs_], data0=ones_row[:, cs_], data1=arg2[:, cs_], initial=0.0, op0=ALU.mult, op1=ALU.add),
                   reads=[ones_row, arg2], writes=[bneg], acc=True)
            tt(aa[:], gi[:], bneg[:], ALU.add, [gi, bneg], [aa])
            op(DVE, lambda e: e.tensor_reduce(out=small[:, 0:4], in_=aa[:].rearrange("p (c l) -> p c l", c=NTT), axis=AX.X, op=ALU.max), reads=[aa], writes=[small])
            for c in range(NTT):
                tt(small[:, 4 + c:5 + c], small[:, c:c + 1], mst[:], ALU.max, [small, mst], [small])
                tt(small[:, 8 + c:9 + c], mst[:], small[:, 4 + c:5 + c], ALU.subtract, [small, mst], [small])
                tt(mst[:], small[:, 4 + c:5 + c], bneg[:, c * 128 + 127:c * 128 + 128], ALU.subtract, [small, bneg], [mst])
                cs_ = slice(c * 128, (c + 1) * 128)
                ts(arg1[:, cs_], aa[:, cs_], small[:, 4 + c:5 + c], LNS, ALU.subtract, ALU.add, [aa, small], [arg1])
                ts(arg2[:, cs_], bneg[:, cs_], small[:, 4 + c:5 + c], None, ALU.subtract, None, [bneg, small], [arg2])
                ts(small[:, 12 + 4 * c:16 + 4 * c], ident4, small[:, 8 + c:9 + c], None, ALU.mult, None, [cm_f, small], [small])
            for c in range(NTT):
                cs_ = slice(c * 128, (c + 1) * 128)
                ps = psum()
                mm(ps[:, 0:4], arg1[:, cs_], ident4, True, True, [arg1, cm_f], [ps])
                mm(ps[:, 4:8], arg2[:, cs_], ident4, True, True, [arg2, cm_f], [ps])
                mm(ps[:, 8:12], ones4, small[:, 12 + 4 * c:16 + 4 * c], True, True, [small, cm_f], [ps])
                act(EX[:, c, :], ps[:, 0:12], AF.Exp, [ps], [EX])

            def conv_chunk(j, ps, full):
                p_ = pre[j % 2]
                a_ = cacc[j % 2]
                act(p_[:, 3:TB + 3], ps[:, :], AF.Identity, [ps, bfm], [p_], bias=bfm[:, j:j + 1])
                cp(p_[:, 0:3], qkcar[:, j, :], [qkcar], [p_])
                if full:
                    ts(a_[:], p_[:, 0:TB], convw[:, j, 0:1], convw[:, j, 4:5], ALU.mult, ALU.add, [p_, convw], [a_])
                    for tap in range(1, 4):
                        stt(a_[:], p_[:, tap:tap + TB], convw[:, j, tap:tap + 1], a_[:], ALU.mult, ALU.add, [p_, convw, a_], [a_])
                    act(qkT[:, j, :], a_[:], AF.Silu, [a_], [qkT])
                cp(qkcar[:, j, :], p_[:, TB:TB + 3], [p_], [qkcar])

            if kind != "pre":
                w = load_cg(win_cols(OFF_Q))
                fm_group(w, 4, lambda cc, ps: conv_chunk(cc, ps, main))
            w = load_cg(win_cols(OFF_K))
            fm_group(w, 4, lambda cc, ps: conv_chunk(4 + cc, ps, True))

            for cg in range(2):
                w = load_cg(win_cols(OFF_V + cg * 512))

                def vcons(t4, ps, cg=cg):
                    tt(vaug[:, t4, 2 * cg:2 * cg + 2, 0:256], ps[:, :].rearrange("p (h c) -> p h c", h=2),
                       btm[:, cg * 512:(cg + 1) * 512].rearrange("p (h c) -> p h c", h=2), ALU.add, [ps, btm], [vaug])
                tm_group(w, vcons)

            if main:
                for cg in range(2):
                    w = load_cg(win_cols(OFF_O + cg * 512))

                    def ocons(t4, ps, cg=cg):
                        o_ = otmp[t4 % 2]
                        tt(o_[:], ps[:, :], btm[:, 1024 + cg * 512:1024 + (cg + 1) * 512], ALU.add, [ps, btm], [o_])
                        act(o_[:], o_[:], AF.Sigmoid, [o_], [o_])
                        tt(gso[:, t4, cg * 512:(cg + 1) * 512], o_[:], mhg[:, cg * 512:(cg + 1) * 512], ALU.mult, [o_, mhg], [gso])
                    tm_group(w, ocons)

            if kind != "pre":
                for cg in range(2):
                    w = load_cg(win_cols(OFF_U + cg * 512))

                    def ucons(cc, ps, cg=cg):
                        j = cg * 4 + cc
                        g = j // 2
                        u_ = ubuf[j % 2]
                        act(u_[:, 15:TB + 15], ps[:, :], AF.Identity, [ps, bfm], [u_], bias=bfm[:, 8 + j:9 + j])
                        cp(u_[:, 0:15], ucar[:, j, :], [ucar], [u_])
                        if main:
                            src = u_
                            sh = 1
                            for stp in range(g + 1):
                                dst = uw[stp % 2]
                                lo = 2 * sh - 1
                                tt(dst[:, lo:TB + 15], src[:, lo:TB + 15], src[:, lo - sh:TB + 15 - sh], ALU.add, [src], [dst])
                                src = dst
                                sh *= 2
                            y_ = yT[g % 2]
                            stt(y_[:, j % 2, :], src[:, 15:TB + 15], 1.0 / WINS[g], u_[:, 15:TB + 15], ALU.mult, ALU.subtract, [src, u_], [y_])
                            if blk_idx == 0:
                                tmpw = uw[(g + 1) % 2]
                                tt(tmpw[:, 0:16], src[:, 15:31], invc[:, g, :], ALU.mult, [src, invc], [tmpw])
                                tt(y_[:, j % 2, 0:16], tmpw[:, 0:16], u_[:, 15:31], ALU.subtract, [tmpw, u_], [y_])
                        cp(ucar[:, j, :], u_[:, TB:TB + 15], [u_], [ucar])
                        if main and j % 2 == 1:
                            y_ = yT[g % 2]
                            for dd in range(2):
                                ps2 = psum()
                                for kc in range(2):
                                    mm(ps2[:, :], wpool[:, g, kc, dd * 128:(dd + 1) * 128], y_[:, kc, :], kc == 0, kc == 1, [wpool, y_], [ps2])
                                act(y2T[:, 2 * g + dd, :], ps2[:, :], AF.Copy, [ps2, poolsc], [y2T], scale=poolsc[:, 2 * g + dd:2 * g + dd + 1])
                    fm_group(w, 4, ucons)

            state["pool"] = [0, 1, 2, 3]
            for c in range(NTT):
                cs_ = slice(c * 128, (c + 1) * 128)
                out_ps = []
                for h in range(NH):
                    kT_h = qkT[:, 4 + h, cs_]
                    qT_h = qkT[:, h, cs_]
                    sk = EX[:, c, h:h + 1]
                    dec = EX[:, c, 8 + h:9 + h]
                    ts(Cs[h][:], Cst[h][:], dec, None, ALU.mult, None, [Cst[h], EX], [Cs[h]])
                    if main:
                        act(Csb[h][:], Cs[h][:], AF.Copy, [Cs[h]], [Csb[h]])
                        ps_s = psum()
                        mm(ps_s[:, 0:128], kT_h, qT_h, True, True, [qkT], [ps_s])
                        S_ = S_sb[h]
                        stt(S_[:], ps_s[:, 0:128], sk, mask_bf, ALU.mult, ALU.mult, [ps_s, EX, cm_bf], [S_])
                    ps_k = psum()
                    kv = ps_k[:, 0:64].bitcast(BF16)
                    tr(kv, kT_h, ident_bf, [qkT, cm_bf], [ps_k])
                    kp_ = kp[h]
                    act(kp_[:], kv, AF.Copy, [ps_k, EX], [kp_], scale=sk)
                    if main:
                        ps_o = banks[4 + h]
                        mm(ps_o[:, 0:DVA], S_[:], vaug[:, c, h, :], True, False, [S_, vaug], [ps_o])
                        mm(ps_o[:, 0:DVA], qT_h, Csb[h][:], False, True, [qkT, Csb[h]], [ps_o])
                        out_ps.append(ps_o)
                    ps_u = psum()
                    mm(ps_u[:, 0:DVA], kp_[:], vaug[:, c, h, :], True, True, [kp_, vaug], [ps_u])
                    tt(Cst[h][:], Cs[h][:], ps_u[:, 0:DVA], ALU.add, [Cs[h], ps_u], [Cst[h]])
                if main:
                    for h in range(NH):
                        op(DVE, lambda e, h=h, p=out_ps[h]: e.bn_stats(out=stat[:, h, :], in_=p[:, 0:256]), reads=[out_ps[h]], writes=[stat])
                        op(DVE, lambda e, h=h: e.bn_aggr(out=mv[:, h, :], in_=stat[:, h, :]), reads=[stat], writes=[mv])
                        act(sm[:, 0, h:h + 1], out_ps[h][:, 256:257], AF.Abs, [out_ps[h]], [sm])
                    tt(sm[:, 0, :], sm[:, 0, :], EX[:, c, 4:8], ALU.max, [sm, EX], [sm])
                    tt(sm[:, 1, :], sm[:, 0, :], sm[:, 0, :], ALU.mult, [sm], [sm])
                    stt(sm[:, 2, :], sm[:, 1, :], LN_EPS, mv[:, :, 1], ALU.mult, ALU.add, [sm, mv], [sm])
                    act(sm[:, 3, :], sm[:, 2, :], AF.Sqrt, [sm], [sm])
                    op(DVE, lambda e: e.reciprocal(out=sm[:, 4, :], in_=sm[:, 3, :]), reads=[sm], writes=[sm])
                    stt(sm[:, 5, :], mv[:, :, 0], -1.0, sm[:, 4, :], ALU.mult, ALU.mult, [sm, mv], [sm])
                    ht_ = htok[c % 2]
                    for h in range(NH):
                        hn = hnrm[h % 2]
                        act(hn[:], out_ps[h][:, 0:256], AF.Identity, [out_ps[h], sm], [hn], bias=sm[:, 5, h:h + 1], scale=sm[:, 4, h:h + 1])
                        tt(ht_[:, h * 256:(h + 1) * 256], hn[:], gso[:, c, h * 256:(h + 1) * 256], ALU.mult, [hn, gso], [ht_])
                    if debug:
                        for h in range(NH):
                            hn = hnrm[h % 2]
                            cp(hn[:], ht_[:, h * 256:(h + 1) * 256], [ht_], [hn])
                            dma(SP, dbg["h"][tok0 - TOK + c * 128:tok0 - TOK + (c + 1) * 128, h * 256:(h + 1) * 256], hn[:], reads=[hn])
                    ps_t = psum()
                    for kc in range(8):
                        tr(ps_t[:, kc * 64:(kc + 1) * 64].bitcast(BF16), ht_[:, kc * 128:(kc + 1) * 128], ident_bf, [ht_, cm_bf], [ps_t])
                    cp(hT[:, :, cs_], ps_t[:, :].bitcast(BF16).rearrange("p (k t) -> p k t", k=8), [ps_t], [hT])
            state["pool"] = list(range(8))
            if not main:
                barrier(skip=(POOL,))
                return

            barrier(skip=(POOL,))
            ar.reset(a_mark)
            sg = [ar.alloc([128, 2, 4, TB], BF16, f"sg{i}") for i in range(2)]
            m1 = [ar.alloc([128, TB], F32, f"m1_{i}") for i in range(2)]
            m2 = [ar.alloc([128, TB], F32, f"m2_{i}") for i in range(2)]
            for dq in range(4):
                sg_ = sg[dq % 2]
                for gi_, off, bcol in ((0, OFF_GM, 16), (1, OFF_GP, 32)):
                    w = load_cg(win_cols(off + dq * 512))

                    def gcons(cc, ps, gi_=gi_, bcol=bcol, dq=dq, sg_=sg_):
                        act(sg_[:, gi_, cc, :], ps[:, :], AF.Sigmoid, [ps, bfm], [sg_], bias=bfm[:, bcol + dq * 4 + cc:bcol + dq * 4 + cc + 1])
                    fm_group(w, 4, gcons)
                w = load_cg(lambda k0, n, dq=dq: wbr_d[dq][:, k0:k0 + n, :])
                for cc in range(4):
                    psa, psp = psum(), psum()
                    for kc in range(8):
                        mm(psa[:, :], w[:, kc, cc * 128:(cc + 1) * 128], hT[:, kc, :], kc == 0, kc == 7, [w, hT], [psa])
                    for kc in range(8):
                        mm(psp[:, :], w[:, 8 + kc, cc * 128:(cc + 1) * 128], y2T[:, kc, :], kc == 0, kc == 7, [w, y2T], [psp])
                    a1, a2 = m1[cc % 2], m2[cc % 2]
                    tt(a1[:], psa[:, :], sg_[:, 0, cc, :], ALU.mult, [psa, sg_], [a1])
                    tt(a2[:], psp[:, :], sg_[:, 1, cc, :], ALU.mult, [psp, sg_], [a2])
                    tt(mrgT[:, dq * 4 + cc, :], a1[:], a2[:], ALU.add, [a1, a2], [mrgT])
                    if debug:
                        tt(a1[:], a1[:], a2[:], ALU.add, [a1, a2], [a1])
                        dma(SP, dbg["mrg"][(dq * 4 + cc) * 128:(dq * 4 + cc + 1) * 128, tok0 - TOK:tok0 - TOK + TB], a1[:], reads=[a1])

            barrier(skip=(POOL,))
            ar.reset(a_mark)
            ln1 = ar.alloc([128, 2, D], F32, "ln1")
            for i in range(2):
                dma(SP, ln1[:, i, :], ln_d[:, i, :], writes=[ln1])
            res = [ar.alloc([128, D], F32, f"res{i}") for i in range(NTT)]
            x1b = ar.alloc([128, D], BF16, "x1b")
            x1T = ar.alloc([128, KC, 128], BF16, "x1T")
            lst = ar.alloc([128, 4, 6], F32, "lst")
            lmv = ar.alloc([128, 8], F32, "lmv")
            rt = ar.alloc([128, 176], F32, "rt")
            rti = ar.alloc([128, 8], U32, "rti")
            abf = ar.alloc([128, 32], BF16, "abf")
            t0 = tok0 - TOK
            for t4 in range(NTT):
                for hh in range(2):
                    dma(SP, res[t4][:, hh * 1024:(hh + 1) * 1024], xtok_d[t0 + t4 * 128:t0 + (t4 + 1) * 128, hh * 1024:(hh + 1) * 1024], writes=[res[t4]])
            for n in range(4):
                w = load_cg(lambda k0, nn, n=n: wout_d[n][:, k0:k0 + nn, :])

                def rcons(t4, ps, n=n):
                    stt(res[t4][:, n * 512:(n + 1) * 512], res[t4][:, n * 512:(n + 1) * 512], ALPHA, ps[:, :], ALU.mult, ALU.add, [res[t4], ps], [res[t4]])
                tm_group(w, rcons, lhs=mrgT)
            for t4 in range(NTT):
                tile_i = blk_idx * NTT + t4
                r_ = res[t4][:]
                if debug:
                    dma(SP, dbg["res"][t0 + t4 * 128:t0 + (t4 + 1) * 128, :], r_, reads=[res[t4]])
                layer_norm(r_, res[t4], ln1, lst, lmv)
                dma(SP, X1_d[t0 + t4 * 128:t0 + (t4 + 1) * 128, :], r_, reads=[res[t4]], writes=[x1d_buf])
                if debug:
                    dma(SP, dbg["x1"][t0 + t4 * 128:t0 + (t4 + 1) * 128, :], r_, reads=[res[t4]])
                act(x1b[:], r_, AF.Copy, [res[t4]], [x1b])
                for half in range(2):
                    ps_t = psum()
                    for kc in range(8):
                        k = half * 8 + kc
                        tr(ps_t[:, kc * 64:(kc + 1) * 64].bitcast(BF16), x1b[:, k * 128:(k + 1) * 128], ident_bf, [x1b, cm_bf], [ps_t])
                    cp(x1T[:, half * 8:(half + 1) * 8, :], ps_t[:, :].bitcast(BF16).rearrange("p (k t) -> p k t", k=8), [ps_t], [x1T])
                ps_l = psum()
                for k in range(KC):
                    mm(ps_l[:, 0:36], x1T[:, k, :], wr[:, k, :], k == 0, k == KC - 1, [x1T, wr], [ps_l])
                routing(tile_i, ps_l, rt, rti, abf, x1b)
                if debug:
                    cp(rt[:, 160:162], gates[:, tile_i, :], [gates], [rt])
                    cp(rt[:, 162:164], slots[:, tile_i, :], [slots], [rt])
                    dma(SP, dbg["rt"][t0 + t4 * 128:t0 + (t4 + 1) * 128, :], rt[:, 160:164], reads=[rt])
            barrier(skip=(POOL,))

        def layer_norm(r_, rT, lnw, lst, lmv):
            ln_stats(r_, rT, lst, lmv)
            ln_norm(r_, rT, lmv)
            ln_affine(r_, rT, lnw)

        def ln_norm(r_, rT, lmv):
            act(r_, r_, AF.Identity, [rT, lmv], [rT], bias=lmv[:, 4:5], scale=lmv[:, 3:4])

        def ln_affine(r_, rT, lnw):
            tt(r_, r_, lnw[:, 0, :], ALU.mult, [rT, lnw], [rT])
            tt(r_, r_, lnw[:, 1, :], ALU.add, [rT, lnw], [rT])

        def ln_stats(r_, rT, lst, lmv):
            for q in range(4):
                op(DVE, lambda e, q=q: e.bn_stats(out=lst[:, q, :], in_=r_[:, q * 512:(q + 1) * 512]), reads=[rT], writes=[lst], acc=True)
            op(DVE, lambda e: e.bn_aggr(out=lmv[:, 0:2], in_=lst[:].rearrange("p a b -> p (a b)")), reads=[lst], writes=[lmv])
            act(lmv[:, 2:3], lmv[:, 1:2], AF.Sqrt, [lmv], [lmv], bias=LN_EPS)
            op(DVE, lambda e: e.reciprocal(out=lmv[:, 3:4], in_=lmv[:, 2:3]), reads=[lmv], writes=[lmv])
            stt(lmv[:, 4:5], lmv[:, 0:1], -1.0, lmv[:, 3:4], ALU.mult, ALU.mult, [lmv], [lmv])

        def routing(ti, ps_l, rt, rti, abf, x1b):
            R, W_ = [ps_l, rt, brt], [rt]
            lg = rt[:, 0:36]
            tt(lg, ps_l[:, 0:36], brt[:], ALU.add, R, W_)
            op(DVE, lambda e: e.tensor_reduce(out=rt[:, 36:37], in_=rt[:, 0:4], axis=AX.X, op=ALU.max), reads=[rt], writes=[rt])
            ts(rt[:, 37:38], rt[:, 36:37], -1.0, None, ALU.mult, None, [rt], [rt])
            op(ACT, lambda e: e.activation(out=rt[:, 40:44], in_=rt[:, 0:4], func=AF.Exp, bias=rt[:, 37:38], accum_out=rt[:, 38:39]), reads=[rt], writes=[rt])
            op(DVE, lambda e: e.reciprocal(out=rt[:, 39:40], in_=rt[:, 38:39]), reads=[rt], writes=[rt])
            ts(rt[:, 44:48], rt[:, 0:4], rt[:, 36:37], None, ALU.is_equal, None, [rt], [rt])
            ts(rt[:, 44:48], rt[:, 44:48], 1e30, -1e30, ALU.mult, ALU.add, [rt], [rt])
            for g in range(4):
                ts(rt[:, 48 + 8 * g:56 + 8 * g], rt[:, 4 + 8 * g:12 + 8 * g], rt[:, 44 + g:45 + g], None, ALU.add, None, [rt], [rt])
            lem = rt[:, 48:80]
            op(DVE, lambda e: e.max(out=rt[:, 80:88], in_=lem), reads=[rt], writes=[rt])
            op(DVE, lambda e: e.max_index(out=rti[:, 0:8], in_max=rt[:, 80:88], in_values=lem), reads=[rt], writes=[rti])
            tt(rt[:, 88:89], rt[:, 80:81], rt[:, 81:82], ALU.subtract, [rt], [rt])
            act(rt[:, 89:90], rt[:, 88:89], AF.Sigmoid, [rt], [rt])
            tt(gates[:, ti, 0:1], rt[:, 39:40], rt[:, 89:90], ALU.mult, [rt], [gates])
            tt(gates[:, ti, 1:2], rt[:, 39:40], gates[:, ti, 0:1], ALU.subtract, [rt, gates], [gates])
            ts(rt[:, 90:122], lem, rt[:, 80:81], None, ALU.is_equal, None, [rt], [rt])
            oh1 = rt[:, 90:122]
            ts(rt[:, 0:32], lem, rt[:, 81:82], None, ALU.is_equal, None, [rt], [rt])
            oh2 = rt[:, 0:32]
            tt(abf[:], oh1, oh2, ALU.add, [rt], [abf])
            ps_c = psum()
            mm(ps_c[:, 0:32], lstr_bf, abf[:], True, False, [cm_bf, abf], [ps_c])
            mm(ps_c[:, 0:32], ones_bf, acum[:], False, True, [cm_bf, acum], [ps_c])
            tt(rt[:, 122:154], ps_c[:, 0:32], oh1, ALU.mult, [ps_c, rt], [rt])
            op(DVE, lambda e: e.tensor_reduce(out=rt[:, 154:155], in_=rt[:, 122:154], axis=AX.X, op=ALU.add), reads=[rt], writes=[rt])
            tt(rt[:, 122:154], ps_c[:, 0:32], oh2, ALU.mult, [ps_c, rt], [rt])
            op(DVE, lambda e: e.tensor_reduce(out=rt[:, 155:156], in_=rt[:, 122:154], axis=AX.X, op=ALU.add), reads=[rt], writes=[rt])
            tt(acum[:], acum[:], abf[:], ALU.add, [acum, abf], [acum])
            cp(rt[:, 156:158], rti[:, 0:2], [rti], [rt])
            stt(rt[:, 158:160], rt[:, 156:158], float(CAP), rt[:, 154:156], ALU.mult, ALU.add, [rt], [rt])
            cp(slots[:, ti, :], rt[:, 158:160], [rt], [slots])
            for k in range(2):
                op(POOL, lambda e, k=k: e.indirect_dma_start(out=XE_d[:, :], out_offset=bass.IndirectOffsetOnAxis(ap=slots[:, ti, k:k + 1], axis=0),
                                                             in_=x1b[:, :], in_offset=None),
                   reads=[x1b, slots], writes=[xe_buf], dma=True, acc=True)

        xe_buf = T(None, "XE")
        x1d_buf = T(None, "X1")
        ye_buf = T(None, "YE")

        for b in range(NPRE):
            block(b * TB, "prelast" if b == NPRE - 1 else "pre", -1)
        for h in range(NH):
            ts(Cst[h][:], Cst[h][:], flag[:, 0:1], None, ALU.mult, None, [Cst[h], flag], [Cst[h]])
        ts(mst[:], mst[:], flag[0:4, 0:1], None, ALU.mult, None, [mst, flag], [mst])
        ts(qkcar[:].rearrange("p a b -> p (a b)"), qkcar[:].rearrange("p a b -> p (a b)"), flag[:, 0:1], None, ALU.mult, None, [qkcar, flag], [qkcar])
        ts(ucar[:].rearrange("p a b -> p (a b)"), ucar[:].rearrange("p a b -> p (a b)"), flag[:, 0:1], None, ALU.mult, None, [ucar, flag], [ucar])
        for b in range(NBLK):
            block(TOK + b * TB, "main", b)

        barrier()
        ar.reset(g_mark)
        gu = [ar.alloc([128, 2, KC, 384], BF16, f"gu{i}") for i in range(2)]
        wdn = [ar.alloc([128, 6, D], BF16, f"wdn{i}") for i in range(2)]
        xg = [ar.alloc([128, D], BF16, f"xg{i}") for i in range(2)]
        xTe = [ar.alloc([128, KC, CAP], BF16, f"xTe{i}") for i in range(2)]
        hidT = [ar.alloc([128, 6, CAP], BF16, f"hidT{i}") for i in range(2)]
        sgt = [ar.alloc([128, CAP], F32, f"sgt{i}") for i in range(2)]
        ysb = [ar.alloc([128, D], F32, f"ysb{i}") for i in range(2)]
        ui = 0
        for e_ in range(NE):
            xTe_ = xTe[e_ % 2]
            hid_ = hidT[e_ % 2]
            for st_ in range(2):
                xg_ = xg[st_]
                r0 = e_ * CAP + st_ * 128
                dma(SP, xg_[:], XE_d[r0:r0 + 128, :], reads=[xe_buf], writes=[xg_])
                for half in range(2):
                    ps_t = psum()
                    for kc in range(8):
                        k = half * 8 + kc
                        tr(ps_t[:, kc * 64:(kc + 1) * 64].bitcast(BF16), xg_[:, k * 128:(k + 1) * 128], ident_bf, [xg_, cm_bf], [ps_t])
                    cp(xTe_[:, half * 8:(half + 1) * 8, st_ * 128:(st_ + 1) * 128], ps_t[:, :].bitcast(BF16).rearrange("p (k t) -> p k t", k=8),
                       [ps_t], [xTe_])
            for half in range(2):
                gu_ = gu[ui % 2]
                ui += 1
                for mi, wsrc in ((0, wg_d), (1, wu_d)):
                    wv = wsrc[e_].rearrange("(kc p) n -> p kc n", p=128)
                    for k0 in range(0, KC, 4):
                        dma(POOL, gu_[:, mi, k0:k0 + 4, :], wv[:, k0:k0 + 4, half * 384:(half + 1) * 384], writes=[gu_])
                for fc in range(3):
                    psg, psu = psum(), psum()
                    for k in range(KC):
                        mm(psg[:, 0:CAP], gu_[:, 0, k, fc * 128:(fc + 1) * 128], xTe_[:, k, :], k == 0, k == KC - 1, [gu_, xTe_], [psg])
                    for k in range(KC):
                        mm(psu[:, 0:CAP], gu_[:, 1, k, fc * 128:(fc + 1) * 128], xTe_[:, k, :], k == 0, k == KC - 1, [gu_, xTe_], [psu])
                    sg_ = sgt[fc % 2]
                    act(sg_[:], psg[:, 0:CAP], AF.Silu, [psg], [sg_])
                    tt(hid_[:, half * 3 + fc, :], sg_[:], psu[:, 0:CAP], ALU.mult, [sg_, psu], [hid_])
            wd_ = wdn[e_ % 2]
            wdv = wd_d[e_].rearrange("(f p) n -> p f n", p=128)
            for f0 in range(0, 6, 2):
                for hh in range(2):
                    dma(POOL, wd_[:, f0:f0 + 2, hh * 1024:(hh + 1) * 1024], wdv[:, f0:f0 + 2, hh * 1024:(hh + 1) * 1024], writes=[wd_])
            for st_ in range(2):
                y_ = ysb[st_]
                for n in range(4):
                    ps = psum()
                    for f in range(6):
                        mm(ps[:, :], hid_[:, f, st_ * 128:(st_ + 1) * 128], wd_[:, f, n * 512:(n + 1) * 512], f == 0, f == 5, [hid_, wd_], [ps])
                    if n % 2 == 0:
                        act(y_[:, n * 512:(n + 1) * 512], ps[:, :], AF.Copy, [ps], [y_], acc=True)
                    else:
                        op(DVE, lambda e, y_=y_, n=n, ps=ps: e.tensor_copy(out=y_[:, n * 512:(n + 1) * 512], in_=ps[:, :]), reads=[ps], writes=[y_], acc=True)
                r0 = e_ * CAP + st_ * 128
                for hh in range(2):
                    dma(SP, YE_d[r0:r0 + 128, hh * 1024:(hh + 1) * 1024], y_[:, hh * 1024:(hh + 1) * 1024], reads=[y_], writes=[ye_buf])

        barrier()
        ar.reset(g_mark)
        wpg = ar.alloc([128, KC, D], BF16, "wpg")
        wpp = ar.alloc([128, 2, D], BF16, "wpp")
        bpg = ar.alloc([128, D], F32, "bpg")
        ln2 = ar.alloc([128, 2, D], F32, "ln2")
        pTt = ar.alloc([128, 2, TOK], BF16, "pTt")
        wpgv = wpg_d.rearrange("(kc p) n -> p kc n", p=128)
        for k in range(KC):
            for hh in range(2):
                dma(POOL, wpg[:, k, hh * 1024:(hh + 1) * 1024], wpgv[:, k, hh * 1024:(hh + 1) * 1024], writes=[wpg])
        wppv = wpp_d.rearrange("(kc p) n -> p kc n", p=128)
        pTv = pT_d.rearrange("(kc p) n -> p kc n", p=128)
        for kc in range(2):
            for hh in range(2):
                dma(POOL, wpp[:, kc, hh * 1024:(hh + 1) * 1024], wppv[:, kc, hh * 1024:(hh + 1) * 1024], writes=[wpp])
                dma(POOL, pTt[:, kc, hh * 1024:(hh + 1) * 1024], pTv[:, kc, hh * 1024:(hh + 1) * 1024], writes=[pTt])
        dma(SP, bpg[:], bpg_d, writes=[bpg])
        for i in range(2):
            dma(SP, ln2[:, i, :], ln_d[:, 2 + i, :], writes=[ln2])
        Y1 = [ar.alloc([128, D], F32, f"Y1_{i}") for i in range(2)]
        Y2 = [ar.alloc([128, D], F32, f"Y2_{i}") for i in range(2)]
        xr = [ar.alloc([128, D], F32, f"xr{i}") for i in range(2)]
        x2b = [ar.alloc([128, D], BF16, f"x2b{i}") for i in range(2)]
        x2T = [ar.alloc([128, KC, 128], BF16, f"x2T{i}") for i in range(2)]
        ot = [ar.alloc([128, D], F32, f"ot{i}") for i in range(2)]
        tg = [ar.alloc([128, 512], F32, f"tg{i}") for i in range(2)]
        lst2 = [ar.alloc([128, 4, 6], F32, f"lst2_{i}") for i in range(2)]
        lmv2 = [ar.alloc([128, 8], F32, f"lmv2_{i}") for i in range(2)]
        finals = []

        def c_a1(ti):
            y1, y2, x_ = Y1[ti % 2], Y2[ti % 2], xr[ti % 2]
            r0 = ti * 128
            for k, yk in ((0, y1), (1, y2)):
                op(POOL, lambda e, k=k, yk=yk, ti=ti: e.indirect_dma_start(out=yk[:, :], out_offset=None, in_=YE_d[:, :],
                                                                          in_offset=bass.IndirectOffsetOnAxis(ap=slots[:, ti, k:k + 1], axis=0)),
                   reads=[ye_buf, slots], writes=[yk], dma=True, acc=False)
            for hh in range(2):
                dma(SP, x_[:, hh * 1024:(hh + 1) * 1024], X1_d[r0:r0 + 128, hh * 1024:(hh + 1) * 1024], reads=[x1d_buf], writes=[x_])
            act(x_[:], x_[:], AF.Copy, [x_], [x_], scale=ALPHA)
            if debug:
                o_ = ot[ti % 2]
                ts(o_[:], y1[:], gates[:, ti, 0:1], None, ALU.mult, None, [y1, gates], [o_])
                stt(o_[:], y2[:], gates[:, ti, 1:2], o_[:], ALU.mult, ALU.add, [y2, gates, o_], [o_])
                dma(SP, dbg["moe"][r0:r0 + 128, :], o_[:], reads=[o_])
            stt(x_[:], y1[:], gates[:, ti, 0:1], x_[:], ALU.mult, ALU.add, [y1, gates, x_], [x_])
            stt(x_[:], y2[:], gates[:, ti, 1:2], x_[:], ALU.mult, ALU.add, [y2, gates, x_], [x_])

        def c_a1b(ti):
            x_ = xr[ti % 2]
            ln_stats(x_[:], x_, lst2[ti % 2], lmv2[ti % 2])

        def c_a1c(ti):
            x_ = xr[ti % 2]
            ln_norm(x_[:], x_, lmv2[ti % 2])

        def c_a1d(ti):
            x_ = xr[ti % 2]
            ln_affine(x_[:], x_, ln2)
            act(x2b[ti % 2][:], x_[:], AF.Copy, [x_], [x2b[ti % 2]])

        def c_a2(ti):
            xb_, xT_ = x2b[ti % 2], x2T[ti % 2]
            for half in range(2):
                ps_t = psum()
                for kc in range(8):
                    k = half * 8 + kc
                    tr(ps_t[:, kc * 64:(kc + 1) * 64].bitcast(BF16), xb_[:, k * 128:(k + 1) * 128], ident_bf, [xb_, cm_bf], [ps_t])
                cp(xT_[:, half * 8:(half + 1) * 8, :], ps_t[:, :].bitcast(BF16).rearrange("p (k t) -> p k t", k=8), [ps_t], [xT_], eng=ACT_OR_DVE[half])

        def c_b(ti, n):
            x_, o_, xT_ = xr[ti % 2], ot[ti % 2], x2T[ti % 2]
            r0 = ti * 128
            if True:
                ns = slice(n * 512, (n + 1) * 512)
                psg, psp = psum(), psum()
                for k in range(KC):
                    mm(psg[:, :], xT_[:, k, :], wpg[:, k, ns], k == 0, k == KC - 1, [xT_, wpg], [psg])
                for kc in range(2):
                    mm(psp[:, :], pTt[:, kc, r0:r0 + 128], wpp[:, kc, ns], kc == 0, kc == 1, [pTt, wpp], [psp])
                t_ = tg[n % 2]
                tt(t_[:], psg[:, :], bpg[:, ns], ALU.add, [psg, bpg], [t_])
                act(t_[:], t_[:], AF.Sigmoid, [t_], [t_])
                tt(t_[:], t_[:], psp[:, :], ALU.mult, [t_, psp], [t_])
                op(DVE, lambda e, o_=o_, t_=t_, x_=x_, ns=ns: e.tensor_tensor(out=o_[:, ns], in0=t_[:], in1=x_[:, ns], op=ALU.add), reads=[t_, x_], writes=[o_], acc=True)
            if n == 3:
                for hh in range(2):
                    finals.append(dma(SP, out_d[r0:r0 + 128, hh * 1024:(hh + 1) * 1024], o_[:, hh * 1024:(hh + 1) * 1024], reads=[o_]))

        ACT_OR_DVE = (DVE, DVE)
        c_a1(0)
        c_a1b(0)
        c_a1c(0)
        c_a1d(0)
        c_a2(0)
        for ti in range(16):
            nxt = ti + 1 < 16
            if nxt:
                c_a1(ti + 1)
            c_b(ti, 0)
            if nxt:
                c_a1b(ti + 1)
            c_b(ti, 1)
            if nxt:
                c_a1c(ti + 1)
            c_b(ti, 2)
            if nxt:
                c_a1d(ti + 1)
            c_b(ti, 3)
            if nxt:
                c_a2(ti + 1)
        s.emit(final_waits=finals)
    return nc


def _prep_shared(inp):
    f = np.float32
    w_in = np.ascontiguousarray(inp["w_in"][0], dtype=f)
    b_in = np.asarray(inp["b_in"][0], dtype=f)
    sh = {}
    def cgl(wc):
        return wc.reshape(wc.shape[0] // 128, 128, wc.shape[1]).transpose(1, 0, 2)
    offs = [OFF_Q, OFF_K, OFF_V, OFF_V + 512, OFF_O, OFF_O + 512, OFF_U, OFF_U + 512]
    offs += [OFF_GM + i * 512 for i in range(4)] + [OFF_GP + i * 512 for i in range(4)]
    sh["w_in_cg"] = np.ascontiguousarray(np.stack([cgl(w_in[:, o:o + 512]) for o in offs]))
    w_if = np.zeros((D, 64), f)
    w_if[:, 0:4] = w_in[:, 3072:3076]
    w_if[:, 32:36] = w_in[:, 3076:3080]
    sh["w_if"] = np.ascontiguousarray(cgl(w_if))
    bfm = np.zeros((128, 56), f)
    bfm[:, 0:8] = b_in[0:1024].reshape(8, 128).T
    bfm[:, 8:16] = b_in[OFF_U:OFF_U + 1024].reshape(8, 128).T
    bfm[:, 16:32] = b_in[OFF_GM:OFF_GM + 2048].reshape(16, 128).T
    bfm[:, 32:48] = b_in[OFF_GP:OFF_GP + 2048].reshape(16, 128).T
    bfm[0:4, 48] = b_in[3072:3076]
    bfm[0:4, 49] = b_in[3076:3080]
    sh["b_fm"] = bfm
    sh["b_tm"] = np.ascontiguousarray(np.broadcast_to(b_in[1024:3072][None, :], (128, 2048)), dtype=f)
    cw = np.zeros((128, 8, 5), f)
    conv_w = np.asarray(inp["conv_w"][0], dtype=f)
    conv_b = np.asarray(inp["conv_b"][0], dtype=f)
    cw[:, :, 0:4] = conv_w.reshape(4, 8, 128).transpose(2, 1, 0)
    cw[:, :, 4] = conv_b.reshape(8, 128).T
    sh["convw"] = cw
    sh["mhg"] = np.ascontiguousarray(np.broadcast_to(np.asarray(inp["mh_g"][0], f)[None, :], (128, 1024)))
    sh["pool_sc"] = np.ascontiguousarray(np.asarray(inp["pool_scale"][0], f).reshape(8, 128).T)
    sh["w_pool"] = np.ascontiguousarray(inp["w_pool"][0], dtype=f)
    wm = np.asarray(inp["w_m_br"][0], dtype=f)
    wp = np.asarray(inp["w_p_br"][0], dtype=f)
    wo = np.asarray(inp["w_out"][0], dtype=f)
    sh["w_br"] = np.ascontiguousarray(np.stack([np.concatenate([cgl(wm[:, q * 512:(q + 1) * 512]), cgl(wp[:, q * 512:(q + 1) * 512])], axis=1) for q in range(4)]))
    sh["w_out_cg"] = np.ascontiguousarray(np.stack([cgl(wo[:, q * 512:(q + 1) * 512]) for q in range(4)]))
    ln = np.stack([inp["ln1_g"][0], inp["ln1_b"][0], inp["ln2_g"][0], inp["ln2_b"][0]], 0).astype(f)
    sh["ln"] = np.ascontiguousarray(np.broadcast_to(ln[None], (128, 4, D)))
    sh["w_r"] = np.ascontiguousarray(np.concatenate([inp["w_rg"][0], inp["w_re"][0]], axis=1), dtype=f)
    b_r = np.concatenate([inp["b_rg"][0], inp["b_re"][0]]).astype(f)
    sh["b_r"] = np.ascontiguousarray(np.broadcast_to(b_r[None, :], (128, 36)))
    sh["w_gate"] = np.ascontiguousarray(inp["w_gate"][0], dtype=f)
    sh["w_up"] = np.ascontiguousarray(inp["w_up"][0], dtype=f)
    sh["w_down"] = np.ascontiguousarray(inp["w_down"][0], dtype=f)
    sh["w_ple_gate"] = np.ascontiguousarray(inp["w_ple_gate"][0], dtype=f)
    sh["b_pg"] = np.ascontiguousarray(np.broadcast_to(np.asarray(inp["b_ple_gate"][0], f)[None, :], (128, D)))
    sh["w_ple_proj"] = np.ascontiguousarray(inp["w_ple_proj"][0], dtype=f)
    cm = np.zeros((128, 4, 128), f)
    cm[:, 0, :] = np.eye(128, dtype=f)
    cm[:, 1, :] = np.triu(np.ones((128, 128), f))
    cm[:, 2, :] = np.triu(np.ones((128, 128), f), k=1)
    cm[:, 3, :] = 1.0
    sh["cmat"] = cm
    return sh


def _prep_core(inp, c):
    f = np.float32
    b, half = c // 2, c % 2
    x = np.asarray(inp["x"], dtype=f)
    p = np.asarray(inp["p"], dtype=f)
    xT = np.zeros((D, 2 * TOK), f)
    if half == 1:
        xT[:, 0:TOK] = x[b, 0:TOK, :].T
    xT[:, TOK:] = x[b, half * TOK:(half + 1) * TOK, :].T
    m = {}
    m["xT"] = xT
    m["xtok"] = np.ascontiguousarray(x[b, half * TOK:(half + 1) * TOK, :])
    m["pT"] = np.ascontiguousarray(p[0, b, half * TOK:(half + 1) * TOK, :].T)
    m["flag"] = np.full((128, 1), float(half), f)
    return m


_CACHE = {}


def kernel(**inputs):
    debug = bool(inputs.pop("_debug", False))
    cores = inputs.pop("_cores", list(range(8)))
    import time as _time
    _t0 = _time.time()
    key = ("nc", debug)
    if key not in _CACHE:
        _CACHE[key] = build_program(debug)
    nc = _CACHE[key]
    _t1 = _time.time()
    sh = _prep_shared(inputs)
    in_maps = []
    for c in cores:
        m = dict(sh)
        m.update(_prep_core(inputs, c))
        in_maps.append(m)
    _t2 = _time.time()
    res = run_bass_kernel_spmd(nc, in_maps, core_ids=list(range(len(cores))))
    print(f"[kernel] build {_t1 - _t0:.1f}s prep {_t2 - _t1:.1f}s run {_time.time() - _t2:.1f}s", flush=True)
    if debug:
        return res.results
    out = np.zeros((4, 4096, D), np.float32)
    for i, c in enumerate(cores):
        b, half = c // 2, c % 2
        out[b, half * TOK:(half + 1) * TOK, :] = res.results[i]["out"]
    return out
```
